# Optimizing a Trainium2 kernel written in Bass

```python
import jax
import jax.numpy as jnp
from jax import lax
import numpy as np

D_MODEL = 1024
BATCH = 8
SEQ = 4096
DEPTH = 2

MLA_HEADS = 4
MLA_NOPE = 128
MLA_ROPE = 64
MLA_V = 128
Q_LORA = 256
KV_LORA = 128
ROPE_THETA = 10000.0
MLA_WIDTH = MLA_HEADS * MLA_V
RWKV_WIDTH = D_MODEL - MLA_WIDTH
RWKV_HEAD = 64
RWKV_HEADS = RWKV_WIDTH // RWKV_HEAD
DECAY_LORA = 64
AAA_LORA = 64
MV_LORA = 32
GATE_LORA = 128
D_FF_DENSE = 2816
N_EXPERTS = 8
TOP_K = 2
D_FF_EXPERT = 3584
Q_BLOCK = 128
NORM_EPS = 1e-6
GN_EPS = 64e-5
L2_EPS = 1e-12
N_DENSE = (DEPTH + 1) // 2
N_MOE = DEPTH // 2
N_VRES = DEPTH - 1
MLA_COLS = Q_LORA + KV_LORA + MLA_ROPE
RWKV_COLS = 3 * RWKV_WIDTH + DECAY_LORA + AAA_LORA + GATE_LORA

kernel_name = 'hybrid_mla_rwkv7_moe_adaln'


def rms_norm(x, g):
    xf = x.astype(jnp.float32)
    y = xf * lax.rsqrt(jnp.mean(xf * xf, axis=-1, keepdims=True) + NORM_EPS)
    return (y * g.astype(jnp.float32)).astype(x.dtype)


def apply_rope(t, positions):
    half = t.shape[-1] // 2
    inv_freq = ROPE_THETA ** (-jnp.arange(half, dtype=jnp.float32) / half)
    ang = positions.astype(jnp.float32)[:, :, None, None] * inv_freq
    cos, sin = jnp.cos(ang), jnp.sin(ang)
    tf = t.astype(jnp.float32)
    t1, t2 = tf[..., :half], tf[..., half:]
    return jnp.concatenate([t1 * cos - t2 * sin, t2 * cos + t1 * sin], axis=-1).astype(t.dtype)


def token_shift(p, mu):
    prev = jnp.pad(p[:, :-1], ((0, 0), (1, 0), (0, 0)))
    return p + (prev - p) * mu


def swiglu(h, wg, wu, wd):
    return (jax.nn.silu(h @ wg) * (h @ wu)) @ wd


def causal_mla_attention(q_nope, q_pe, k_nope, k_pe, v):
    bsz, seq, heads, _ = q_nope.shape
    nb = seq // Q_BLOCK
    scale = (MLA_NOPE + MLA_ROPE) ** -0.5
    key_idx = jnp.arange(seq)

    def to_blocks(t):
        return jnp.moveaxis(t.reshape(bsz, nb, Q_BLOCK, *t.shape[2:]), 1, 0)

    def one_block(args):
        qn, qp, blk = args
        s = (jnp.einsum('bqhd,bkhd->bhqk', qn, k_nope)
             + jnp.einsum('bqhr,bkr->bhqk', qp, k_pe))
        s = s.astype(jnp.float32) * scale
        q_idx = blk * Q_BLOCK + jnp.arange(Q_BLOCK)
        s = jnp.where(key_idx[None, :] <= q_idx[:, None], s, -jnp.inf)
        prob = jax.nn.softmax(s, axis=-1).astype(v.dtype)
        return jnp.einsum('bhqk,bkhd->bqhd', prob, v)

    out = lax.map(one_block, (to_blocks(q_nope), to_blocks(q_pe), jnp.arange(nb)))
    return jnp.moveaxis(out, 0, 1).reshape(bsz, seq, heads * v.shape[-1])


def mla_mixer(p, positions, g_q, w_uq, g_kv, w_ukv, g_out):
    bsz, seq, _ = p.shape
    c_q = rms_norm(p[..., :Q_LORA], g_q)
    c_kv = rms_norm(p[..., Q_LORA:Q_LORA + KV_LORA], g_kv)
    k_pe = apply_rope(p[..., Q_LORA + KV_LORA:MLA_COLS][:, :, None, :], positions)[:, :, 0]
    q = (c_q @ w_uq).reshape(bsz, seq, MLA_HEADS, MLA_NOPE + MLA_ROPE)
    q_nope = q[..., :MLA_NOPE]
    q_pe = apply_rope(q[..., MLA_NOPE:], positions)
    kv = (c_kv @ w_ukv).reshape(bsz, seq, MLA_HEADS, MLA_NOPE + MLA_V)
    k_nope, v = kv[..., :MLA_NOPE], kv[..., MLA_NOPE:]
    o = causal_mla_attention(q_nope, q_pe, k_nope, k_pe, v)
    return rms_norm(o, g_out)


def rwkv7_recurrence(r, decay, k, v, a, b):
    bsz, seq, heads, n = r.shape

    def step(state, inp):
        r_t, w_t, k_t, v_t, a_t, b_t = inp
        sa = jnp.einsum('bhij,bhj->bhi', state, a_t)
        state = (state * w_t[:, :, None, :] + sa[..., None] * b_t[:, :, None, :]
                 + v_t[..., None] * k_t[:, :, None, :])
        return state, jnp.einsum('bhij,bhj->bhi', state, r_t)

    xs = tuple(jnp.moveaxis(t.astype(jnp.float32), 1, 0) for t in (r, decay, k, v, a, b))
    s0 = jnp.zeros((bsz, heads, n, n), jnp.float32)
    _, y = lax.scan(step, s0, xs)
    return jnp.moveaxis(y, 0, 1)


def rwkv7_mixer(p, v_first, w0, w2, a0, a2, g2, k_k, k_a, r_k, ln_w, ln_b, v0, v2):
    bsz, seq, _ = p.shape
    sizes = [RWKV_WIDTH, RWKV_WIDTH, RWKV_WIDTH, DECAY_LORA, AAA_LORA, GATE_LORA]
    offs = [int(o) for o in np.cumsum([0] + sizes)]
    r, k, v, w_d, a_d, g_d = (p[..., offs[i]:offs[i + 1]] for i in range(6))

    w_log = -jax.nn.softplus(-(w0 + jnp.tanh(w_d) @ w2)) - 0.5
    decay = jnp.exp(-jnp.exp(w_log.astype(jnp.float32)))
    if v_first is None:
        v_first = v
    else:
        v = v + (v_first - v) * jax.nn.sigmoid(v0 + p[..., offs[6]:] @ v2)
    a = jax.nn.sigmoid(a0 + a_d @ a2)
    g = jax.nn.sigmoid(g_d) @ g2

    def heads(t):
        return t.reshape(bsz, seq, RWKV_HEADS, RWKV_HEAD)

    kk = heads(k * k_k).astype(jnp.float32)
    kk = kk / jnp.maximum(jnp.sqrt(jnp.sum(kk * kk, axis=-1, keepdims=True)), L2_EPS)
    k = k * (1 + (a - 1) * k_a)
    rh, kh, vh, ah = heads(r), heads(k), heads(v), heads(a)
    y = rwkv7_recurrence(rh, heads(decay), kh, vh, -kk, kk * ah)
    mean = jnp.mean(y, axis=-1, keepdims=True)
    var = jnp.mean(jnp.square(y - mean), axis=-1, keepdims=True)
    y = (y - mean) * lax.rsqrt(var + GN_EPS)
    y = y * ln_w.reshape(RWKV_HEADS, RWKV_HEAD) + ln_b.reshape(RWKV_HEADS, RWKV_HEAD)
    bonus = jnp.sum(rh * kh * r_k, axis=-1, keepdims=True) * vh
    out = (y.astype(p.dtype) + bonus).reshape(bsz, seq, RWKV_WIDTH) * g
    return out, v_first


def moe_swiglu(h, w_router, b_router, wg, wu, wd):
    logits = (h @ w_router + b_router).astype(jnp.float32)
    top_val, top_idx = lax.top_k(logits, TOP_K)
    top_w = jax.nn.softmax(top_val, axis=-1)
    gates = jnp.sum(jax.nn.one_hot(top_idx, N_EXPERTS, dtype=jnp.float32) * top_w[..., None],
                    axis=-2).astype(h.dtype)
    out = jnp.zeros_like(h)
    for e in range(N_EXPERTS):
        out = out + gates[..., e:e + 1] * swiglu(h, wg[e], wu[e], wd[e])
    return out


def setup_inputs(seed: int = 0) -> dict:
    key = jax.random.key(seed)
    ks = iter(jax.random.split(key, 64))
    D = D_MODEL

    def nrm(shape, scale):
        return jax.random.normal(next(ks), shape, jnp.float32) * scale

    def gain(shape):
        return 1.0 + nrm(shape, 0.02)

    def unif(shape, lo, hi):
        return jax.random.uniform(next(ks), shape, jnp.float32, lo, hi)

    pos_offset = jax.random.randint(next(ks), (BATCH, 1), 0, 1024, dtype=jnp.int32)
    positions = pos_offset + jnp.arange(SEQ, dtype=jnp.int32)[None, :]
    return {
        'x': nrm((BATCH, SEQ, D), 1.0),
        'c': nrm((BATCH, D), 1.0),
        'positions': positions,
        'w_ada': nrm((DEPTH, D, 6 * D), 0.5 * D ** -0.5),
        'b_ada': nrm((DEPTH, 6 * D), 0.01),
        'g_norm_mix': gain((DEPTH, D)),
        'g_norm_ffn': gain((DEPTH, D)),
        'w_in': nrm((DEPTH, D, MLA_COLS + RWKV_COLS), D ** -0.5),
        'w_in_vres': nrm((N_VRES, D, MV_LORA), D ** -0.5),
        'g_q_norm': gain((DEPTH, Q_LORA)),
        'w_uq': nrm((DEPTH, Q_LORA, MLA_HEADS * (MLA_NOPE + MLA_ROPE)), Q_LORA ** -0.5),
        'g_kv_norm': gain((DEPTH, KV_LORA)),
        'w_ukv': nrm((DEPTH, KV_LORA, MLA_HEADS * (MLA_NOPE + MLA_V)), KV_LORA ** -0.5),
        'g_attn_out': gain((DEPTH, MLA_WIDTH)),
        'mu_shift': unif((DEPTH, RWKV_COLS), 0.0, 1.0),
        'mu_shift_vres': unif((N_VRES, MV_LORA), 0.0, 1.0),
        'w0': unif((DEPTH, RWKV_WIDTH), -6.0, -1.0),
        'w2': nrm((DEPTH, DECAY_LORA, RWKV_WIDTH), 0.5 * DECAY_LORA ** -0.5),
        'a0': nrm((DEPTH, RWKV_WIDTH), 0.1),
        'a2': nrm((DEPTH, AAA_LORA, RWKV_WIDTH), 0.5 * AAA_LORA ** -0.5),
        'g2': nrm((DEPTH, GATE_LORA, RWKV_WIDTH), GATE_LORA ** -0.5),
        'v0': nrm((N_VRES, RWKV_WIDTH), 0.1),
        'v2': nrm((N_VRES, MV_LORA, RWKV_WIDTH), 0.5 * MV_LORA ** -0.5),
        'k_k': 1.0 + nrm((DEPTH, RWKV_WIDTH), 0.1),
        'k_a': 1.0 + nrm((DEPTH, RWKV_WIDTH), 0.1),
        'r_k': nrm((DEPTH, RWKV_HEADS, RWKV_HEAD), 0.1),
        'ln_x_w': gain((DEPTH, RWKV_WIDTH)),
        'ln_x_b': nrm((DEPTH, RWKV_WIDTH), 0.01),
        'w_out': nrm((DEPTH, D, D), D ** -0.5),
        'w_ffn_gate': nrm((N_DENSE, D, D_FF_DENSE), D ** -0.5),
        'w_ffn_up': nrm((N_DENSE, D, D_FF_DENSE), D ** -0.5),
        'w_ffn_down': nrm((N_DENSE, D_FF_DENSE, D), D_FF_DENSE ** -0.5),
        'w_router': nrm((N_MOE, D, N_EXPERTS), D ** -0.5),
        'b_router': nrm((N_MOE, N_EXPERTS), 0.01),
        'w_exp_gate': nrm((N_MOE, N_EXPERTS, D, D_FF_EXPERT), D ** -0.5),
        'w_exp_up': nrm((N_MOE, N_EXPERTS, D, D_FF_EXPERT), D ** -0.5),
        'w_exp_down': nrm((N_MOE, N_EXPERTS, D_FF_EXPERT, D), D_FF_EXPERT ** -0.5),
        'g_final': gain((D,)),
    }


def reference(x, c, positions, w_ada, b_ada, g_norm_mix, g_norm_ffn, w_in, w_in_vres,
              g_q_norm, w_uq, g_kv_norm, w_ukv, g_attn_out, mu_shift, mu_shift_vres,
              w0, w2, a0, a2, g2, v0, v2, k_k, k_a, r_k, ln_x_w, ln_x_b, w_out,
              w_ffn_gate, w_ffn_up, w_ffn_down, w_router, b_router,
              w_exp_gate, w_exp_up, w_exp_down, g_final):
    cond = jax.nn.silu(c)
    v_first = None
    for l in range(DEPTH):
        mod = cond @ w_ada[l] + b_ada[l]
        sh1, sc1, gt1, sh2, sc2, gt2 = (m[:, None, :] for m in jnp.split(mod, 6, axis=-1))

        h = rms_norm(x, g_norm_mix[l]) * (1 + sc1) + sh1
        if l == 0:
            w_cat, mu = w_in[l], mu_shift[l]
            v0_l, v2_l = None, None
        else:
            w_cat = jnp.concatenate([w_in[l], w_in_vres[l - 1]], axis=-1)
            mu = jnp.concatenate([mu_shift[l], mu_shift_vres[l - 1]], axis=-1)
            v0_l, v2_l = v0[l - 1], v2[l - 1]
        proj = h @ w_cat
        y_mla = mla_mixer(proj[..., :MLA_COLS], positions, g_q_norm[l], w_uq[l],
                          g_kv_norm[l], w_ukv[l], g_attn_out[l])
        y_rwkv, v_first = rwkv7_mixer(token_shift(proj[..., MLA_COLS:], mu), v_first,
                                      w0[l], w2[l], a0[l], a2[l], g2[l], k_k[l], k_a[l],
                                      r_k[l], ln_x_w[l], ln_x_b[l], v0_l, v2_l)
        x = x + gt1 * (jnp.concatenate([y_mla, y_rwkv], axis=-1) @ w_out[l])

        h = rms_norm(x, g_norm_ffn[l]) * (1 + sc2) + sh2
        i = l // 2
        if l % 2 == 0:
            f = swiglu(h, w_ffn_gate[i], w_ffn_up[i], w_ffn_down[i])
        else:
            f = moe_swiglu(h, w_router[i], b_router[i], w_exp_gate[i], w_exp_up[i], w_exp_down[i])
        x = x + gt2 * f
    return rms_norm(x, g_final)
```

```python
import math
from contextlib import ExitStack

import numpy as np
import concourse.bass as bass
import concourse.mybir as mybir
from concourse.bass_utils import run_bass_kernel_spmd

F32 = mybir.dt.float32
BF16 = mybir.dt.bfloat16
I32 = mybir.dt.int32
AF = mybir.ActivationFunctionType
ALU = mybir.AluOpType
AX = mybir.AxisListType

S = 4096
D = 1024
NT = S // 128
NG = S // 512
DEPTH = 2
MLA_COLS = 448
NDS = 40
NORM_EPS = 1e-6
GN_EPS = 64e-5
SCALE = (128 + 64) ** -0.5
DEC_C = math.exp(-0.5)


class Buf:
    __slots__ = ("w", "r")

    def __init__(self):
        self.w = []
        self.r = []


class Em:
    def __init__(self, nc):
        self.nc = nc
        self.eng = {"pe": nc.tensor, "act": nc.scalar, "dve": nc.vector, "pool": nc.gpsimd, "sp": nc.sync}
        self.sem = {e: nc.semaphore("s_" + e).__enter__() for e in ("pe", "act", "dve", "pool")}
        self.cnt = {e: 0 for e in self.sem}
        self.seen = {e: {} for e in self.eng}
        self.dsems = [nc.semaphore("d%d" % i).__enter__() for i in range(NDS)]
        self.dtot = [0] * NDS
        self.dnext = 0

    def _wait(self, e, tok):
        key, val, sem, src = tok
        if src == "pe" and e == "pe":
            return
        if self.seen[e].get(key, 0) >= val:
            return
        self.eng[e].wait_ge(sem, val)
        self.seen[e][key] = val

    def _deps(self, e, r, w, is_dma=False):
        for b in r:
            for t in b.w:
                self._wait(e, t)
        for b in w:
            for t in b.r:
                self._wait(e, t)
            for t in b.w:
                if is_dma and t[3] == "dma" and not b.r:
                    continue
                self._wait(e, t)

    def _commit(self, tok, r, w, is_dma=False):
        for b in r:
            b.r = [t for t in b.r if t[0] != tok[0]] + [tok]
        for b in w:
            if is_dma and not b.r and b.w and all(t[3] == "dma" for t in b.w):
                b.w = [t for t in b.w if t[0] != tok[0]] + [tok]
            else:
                b.w = [tok]
            b.r = []

    def op(self, e, fn, r=(), w=()):
        self._deps(e, r, w)
        ins = fn(self.eng[e])
        self.cnt[e] += 1
        ins.then_inc(self.sem[e], 1)
        self._commit((e, self.cnt[e], self.sem[e], e), r, w)

    def dma(self, q, out, in_, r=(), w=(), **kw):
        k = self.dnext
        self.dnext = (k + 1) % NDS
        if self.dtot[k]:
            self._wait(q, ("d%d" % k, self.dtot[k], self.dsems[k], "dma"))
        self._deps(q, r, w, is_dma=True)
        ins = self.eng[q].dma_start(out=out, in_=in_, **kw)
        self.dtot[k] += 16
        ins.then_inc(self.dsems[k], 16)
        self._commit(("d%d" % k, self.dtot[k], self.dsems[k], "dma"), r, w, is_dma=True)

    def barrier(self):
        for e in self.eng:
            for f in self.sem:
                if self.cnt[f]:
                    self._wait(e, (f, self.cnt[f], self.sem[f], "bar"))
            for k in range(NDS):
                if self.dtot[k]:
                    self._wait(e, ("d%d" % k, self.dtot[k], self.dsems[k], "dma"))


def build(dbg=None, depth=DEPTH):
    nc = bass.Bass("TRN2", target_bir_lowering=False)
    em = Em(nc)

    def din(name, shape, dt=F32):
        return nc.dram_tensor(name, list(shape), dt, kind="ExternalInput").ap()

    def dscr(name, shape, dt=F32):
        return nc.dram_tensor(name, list(shape), dt).ap()

    x_in = din("x", [S, D])
    c_in = din("c", [1, D])
    pos_in = din("positions", [1, S], I32)
    w_ada = din("w_ada", [2, D, 6 * D])
    b_ada = din("b_ada", [2, 6 * D])
    g_norm_mix = din("g_norm_mix", [2, D])
    g_norm_ffn = din("g_norm_ffn", [2, D])
    w_in = din("w_in", [2, D, 2240])
    w_in_vres = din("w_in_vres", [1, D, 32])
    g_q_norm = din("g_q_norm", [2, 256])
    w_uq = din("w_uq", [2, 256, 768])
    g_kv_norm = din("g_kv_norm", [2, 128])
    w_ukv = din("w_ukv", [2, 128, 1024])
    g_attn_out = din("g_attn_out", [2, 512])
    mu_shift = din("mu_shift", [2, 1792])
    mu_shift_vres = din("mu_shift_vres", [1, 32])
    w0_in = din("w0", [2, 512])
    w2_in = din("w2", [2, 64, 512])
    a0_in = din("a0", [2, 512])
    a2_in = din("a2", [2, 64, 512])
    g2_in = din("g2", [2, 128, 512])
    v0_in = din("v0", [1, 512])
    v2_in = din("v2", [1, 32, 512])
    k_k_in = din("k_k", [2, 512])
    k_a_in = din("k_a", [2, 512])
    r_k_in = din("r_k", [2, 512])
    ln_w_in = din("ln_x_w", [2, 512])
    ln_b_in = din("ln_x_b", [2, 512])
    w_out = din("w_out", [2, D, D])
    w_ffn_gate = din("w_ffn_gate", [1, D, 2816])
    w_ffn_up = din("w_ffn_up", [1, D, 2816])
    w_ffn_down = din("w_ffn_down", [1, 2816, D])
    if depth > 1:
        w_router = din("w_router", [1, D, 8])
        b_router = din("b_router", [1, 8])
        w_exp_gate = din("w_exp_gate", [8, D, 3584])
        w_exp_up = din("w_exp_up", [8, D, 3584])
        w_exp_down = din("w_exp_down", [8, 3584, D])
    g_final = din("g_final", [1, D])
    c_ident = din("c_ident", [128, 128])
    c_mincl = din("c_mincl", [128, 128])
    c_mstrict = din("c_mstrict", [128, 128])
    c_blk = din("c_blk", [128, 128])
    c_mstrictL = din("c_mstrictL", [128, 128])
    c_hsel = din("c_hsel", [128, 2])
    c_invf = din("c_invf", [64, 1])
    c_sgn = din("c_sgn", [64, 1])

    out_d = nc.dram_tensor("out", [S, D], F32, kind="ExternalOutput").ap()
    if dbg:
        dbg_d = nc.dram_tensor("dbg", list(dbg[1]), F32, kind="ExternalOutput").ap()

    xres = dscr("xres", [S, D])
    qn_d = dscr("qn_d", [4, 128, S], BF16)
    qpe_d = dscr("qpe_d", [4, 64, S], BF16)
    qsq_d = dscr("qsq_d", [4, S])
    kn_d = dscr("kn_d", [4, 128, S], BF16)
    kpe_d = dscr("kpe_d", [64, S], BF16)
    v_d = dscr("v_d", [S, 512], BF16)
    ycat_d = dscr("ycat_d", [8, 128, S], BF16)
    vfirst_d = dscr("vfirst_d", [4, 128, S])
    rw_d = dscr("rw_d", [15, 128, S])
    B_rw = Buf()
    B_x = Buf(); B_xres = Buf(); B_q = Buf(); B_k = Buf(); B_ycat = Buf(); B_vf = Buf(); B_out = Buf(); B_dbg = Buf()

    es0 = ExitStack()

    uid = [0]

    def sb(es, name, shape, dt=F32):
        uid[0] += 1
        return es.enter_context(nc.sbuf_tensor("%s_u%d" % (name, uid[0]), list(shape), dt))

    ps = [es0.enter_context(nc.psum_tensor("ps%d" % i, [128, 512], F32)) for i in range(8)]
    Bps = [Buf() for _ in range(8)]
    psrr = [0]

    def nps():
        i = psrr[0]
        psrr[0] = (i + 1) % 8
        return ps[i], Bps[i]

    ident_f = sb(es0, "ident_f", [128, 128]); ident_b = sb(es0, "ident_b", [128, 128], BF16)
    mincl = sb(es0, "mincl", [128, 128]); mstrict = sb(es0, "mstrict", [128, 128]); mincl_b = sb(es0, "mincl_b", [128, 128], BF16)
    mstrictL = sb(es0, "mstrictL", [128, 128]); hsel = sb(es0, "hsel", [128, 2]); gneps = sb(es0, "gneps", [128, 1])
    blk_f = sb(es0, "blk_f", [128, 128]); ones_f = sb(es0, "ones_f", [128, 128]); ones_b = sb(es0, "ones_b", [128, 128], BF16)
    invf = sb(es0, "invf", [64, 1]); sgn = sb(es0, "sgn", [64, 1])
    negpi = sb(es0, "negpi", [128, 1]); epsn = sb(es0, "epsn", [128, 1])
    Bc = Buf()
    for t, s_ in ((ident_f, c_ident), (mincl, c_mincl), (mstrict, c_mstrict), (blk_f, c_blk)):
        em.dma("sp", t[:, :], s_[:, :], w=[Bc])
    em.dma("sp", invf[:, :], c_invf[:, :], w=[Bc])
    em.dma("sp", mstrictL[:, :], c_mstrictL[:, :], w=[Bc])
    em.dma("sp", hsel[:, :], c_hsel[:, :], w=[Bc])
    em.op("dve", lambda e: e.memset(gneps[:, :], GN_EPS), w=[Bc])
    em.dma("sp", sgn[:, :], c_sgn[:, :], w=[Bc])
    em.op("dve", lambda e: e.tensor_copy(ident_b[:, :], ident_f[:, :]), r=[Bc], w=[Bc])
    em.op("dve", lambda e: e.tensor_copy(mincl_b[:, :], mincl[:, :]), r=[Bc], w=[Bc])
    em.op("dve", lambda e: e.memset(ones_f[:, :], 1.0), w=[Bc])
    em.op("dve", lambda e: e.memset(ones_b[:, :], 1.0), w=[Bc])
    em.op("dve", lambda e: e.memset(negpi[:, :], -math.pi), w=[Bc])
    em.op("dve", lambda e: e.memset(epsn[:, :], NORM_EPS), w=[Bc])

    mod_col = [sb(es0, "mod_col%d" % l, [128, 48]) for l in range(depth)]
    gs1 = [sb(es0, "gs1_%d" % l, [128, 8]) for l in range(depth)]
    gs2 = [sb(es0, "gs2_%d" % l, [128, 8]) for l in range(depth)]
    gt1_bc = [sb(es0, "gt1bc%d" % l, [128, D]) for l in range(depth)]
    gt2_bc = [sb(es0, "gt2bc%d" % l, [128, D]) for l in range(depth)]
    gs2_bc = sb(es0, "gs2bc", [128, D]) if depth > 1 else None
    sh2_bc = sb(es0, "sh2bc", [128, D]) if depth > 1 else None
    Bmod = Buf()

    with ExitStack() as es:
        ccol = sb(es, "ccol", [128, 8]); cond = sb(es, "cond", [128, 8])
        cond_bc = sb(es, "cond_bc", [128, 8, 128])
        wada_t = [sb(es, "wada%d" % i, [128, 8, 512]) for i in range(4)]
        Bwada = [Buf() for _ in range(4)]
        mod_bc = sb(es, "mod_bc", [128, 6 * D]); bias_bc = sb(es, "bias_bc", [128, 6 * D])
        tmpd = sb(es, "tmpd", [128, 48, 128])
        gcol = sb(es, "gcol", [128, 8])
        Bl = Buf()
        em.dma("sp", ccol[:, :], c_in[0, :].rearrange("(k p) -> p k", p=128), w=[Bl], allow_slow_non_contiguous=True)
        em.op("act", lambda e: e.activation(out=cond[:, :], in_=ccol[:, :], func=AF.Silu), r=[Bl], w=[Bl])
        em.op("dve", lambda e: e.tensor_copy(cond_bc[:, :, :], cond[:, :].unsqueeze(2).to_broadcast([128, 8, 128])), r=[Bl], w=[Bl])
        for l in range(depth):
            em.dma("sp", bias_bc[:, :], b_ada[l:l + 1, :].partition_broadcast(128) if False else b_ada[l:l + 1, :].to_broadcast([128, 6 * D]), w=[Bl])
            for gI in range(12):
                wt, bw = wada_t[gI % 4], Bwada[gI % 4]
                em.dma("sp", wt[:, 0:4, :], w_ada[l, 0:512, gI * 512:(gI + 1) * 512].rearrange("(k p) n -> p k n", p=128), w=[bw])
                em.dma("sp", wt[:, 4:8, :], w_ada[l, 512:1024, gI * 512:(gI + 1) * 512].rearrange("(k p) n -> p k n", p=128), w=[bw])
                pt, bp = nps()
                for k in range(8):
                    em.op("pe", lambda e, k=k: e.matmul(pt[:, :], lhsT=cond_bc[:, k, :], rhs=wt[:, k, :], start=(k == 0), stop=(k == 7)), r=[Bl, bw], w=[bp])
                em.op("dve", lambda e: e.tensor_tensor(out=mod_bc[:, gI * 512:(gI + 1) * 512], in0=pt[:, :], in1=bias_bc[:, gI * 512:(gI + 1) * 512], op=ALU.add), r=[bp, Bl], w=[Bl])
            em.op("dve", lambda e: e.tensor_tensor(out=tmpd[:, :, :], in0=mod_bc[:, :].rearrange("p (a b) -> p a b", b=128), in1=ident_f[:, :].unsqueeze(1).to_broadcast([128, 48, 128]), op=ALU.mult), r=[Bl, Bc], w=[Bl])
            em.op("dve", lambda e: e.tensor_reduce(out=mod_col[l][:, :], in_=tmpd[:, :, :], axis=AX.X, op=ALU.add), r=[Bl], w=[Bmod])
            for (gsrc, scv, gdst) in ((g_norm_mix, 1, gs1[l]), (g_norm_ffn, 4, gs2[l])):
                em.dma("sp", gcol[:, :], gsrc[l, :].rearrange("(k p) -> p k", p=128), w=[Bl], allow_slow_non_contiguous=True)
                em.op("dve", lambda e: e.scalar_tensor_tensor(out=gdst[:, :], in0=mod_col[l][:, scv * 8:scv * 8 + 8], scalar=1.0, in1=gcol[:, :], op0=ALU.add, op1=ALU.mult), r=[Bl, Bmod], w=[Bmod])
            em.op("dve", lambda e: e.tensor_copy(gt1_bc[l][:, :], mod_bc[:, 2 * D:3 * D]), r=[Bl], w=[Bmod])
            em.op("dve", lambda e: e.tensor_copy(gt2_bc[l][:, :], mod_bc[:, 5 * D:6 * D]), r=[Bl], w=[Bmod])
            if l == 1:
                gbc = sb(es, "gbc", [128, D])
                em.dma("sp", gbc[:, :], g_norm_ffn[l:l + 1, :].to_broadcast([128, D]), w=[Bl])
                em.op("dve", lambda e: e.scalar_tensor_tensor(out=gs2_bc[:, :], in0=mod_bc[:, 4 * D:5 * D], scalar=1.0, in1=gbc[:, :], op0=ALU.add, op1=ALU.mult), r=[Bl], w=[Bmod])
                em.op("dve", lambda e: e.tensor_copy(sh2_bc[:, :], mod_bc[:, 3 * D:4 * D]), r=[Bl], w=[Bmod])
        em.barrier()

    def rstd_op(dst, src, scale, rd, wr, rows=128):
        em.op("act", lambda e: e.activation(out=dst, in_=src, func=AF.Sqrt, bias=epsn[0:rows, 0:1], scale=scale), r=list(rd) + [Bc], w=list(wr))
        em.op("dve", lambda e: e.reciprocal(out=dst, in_=dst), r=list(wr), w=list(wr))

    def rmsnorm_T(es_pool, xt, Bxt, gs_col, sh_col, hT_dst, Bh, col0, tagbuf):
        junk, ssum, rstd, xn, Bt = tagbuf
        em.op("act", lambda e: e.activation(out=junk[:, :], in_=xt[:, :], func=AF.Square, accum_out=ssum[:, :]), r=[Bxt], w=[Bt])
        rstd_op(rstd[:, :], ssum[:, :], 1.0 / D, [Bt], [Bt])
        em.op("act", lambda e: e.activation(out=xn[:, :], in_=xt[:, :], func=AF.Copy, scale=rstd[:, 0:1]), r=[Bxt, Bt], w=[Bt])
        pt, bp = nps()
        ptb = pt[:, :].bitcast(BF16)
        for k in range(8):
            em.op("pe", lambda e, k=k: e.transpose(ptb[:, k * 128:(k + 1) * 128], xn[:, k * 128:(k + 1) * 128], ident_b[:, :]), r=[Bt, Bc], w=[bp])
        for k in range(8):
            eng = "dve" if k % 2 == 0 else "act"
            if eng == "dve":
                em.op("dve", lambda e, k=k: e.tensor_scalar(out=hT_dst[:, k, col0:col0 + 128], in0=ptb[:, k * 128:(k + 1) * 128], scalar1=gs_col[:, k:k + 1], scalar2=sh_col[:, k:k + 1], op0=ALU.mult, op1=ALU.add), r=[bp, Bmod], w=[Bh])
            else:
                em.op("act", lambda e, k=k: e.activation(out=hT_dst[:, k, col0:col0 + 128], in_=ptb[:, k * 128:(k + 1) * 128], func=AF.Identity, scale=gs_col[:, k:k + 1], bias=sh_col[:, k:k + 1]), r=[bp, Bmod], w=[Bh])

    Bwbf = Buf()
    wbf = {}

    def conv_w(name, src2d):
        R_, C_ = src2d.shape
        dst = dscr(name, [R_, C_], BF16)
        for r0 in range(0, R_, 256):
            r1 = min(R_, r0 + 256)
            em.dma("pool", dst[r0:r1, :], src2d[r0:r1, :], w=[Bwbf])
        return dst
    wbf["fg"] = conv_w("wbf_fg", w_ffn_gate[0]); wbf["fu"] = conv_w("wbf_fu", w_ffn_up[0]); wbf["fd"] = conv_w("wbf_fd", w_ffn_down[0])
    if depth > 1:
        wbf["eg"] = [conv_w("wbf_eg%d" % e_, w_exp_gate[e_]) for e_ in range(8)]
        wbf["eu"] = [conv_w("wbf_eu%d" % e_, w_exp_up[e_]) for e_ in range(8)]
        wbf["ed"] = [conv_w("wbf_ed%d" % e_, w_exp_down[e_]) for e_ in range(8)]

    for l in range(depth):
        x_src = x_in if l == 0 else xres
        Bxsrc = B_x if l == 0 else B_xres
        sh1_col = mod_col[l][:, 0:8]
        sh2_col = mod_col[l][:, 24:32]
        NCH = 18 if l == 0 else 19
        with ExitStack() as es:
            wcat = sb(es, "wcat", [128, 8, 19 * 128], BF16); Bw = Buf()
            wst = sb(es, "wst", [128, 8, 512]); Bwst = Buf()

            def load_cols(dst_c0, src_ap_fn, ncols):
                em.dma("sp", wst[:, :, 0:ncols], src_ap_fn, w=[Bwst])
                em.op("pool", lambda e: e.tensor_copy(wcat[:, :, dst_c0:dst_c0 + ncols], wst[:, :, 0:ncols]), r=[Bwst], w=[Bw])

            wv = w_in[l].rearrange("(k p) n -> p k n", p=128)
            load_cols(0, wv[:, :, 0:448], 448)
            load_cols(448, wv[:, :, 416:448], 32)
            load_cols(480, wv[:, :, 384:416], 32)
            for j in range(3):
                load_cols(512 + j * 512, wv[:, :, 448 + j * 512:448 + (j + 1) * 512], 512)
            load_cols(2048, wv[:, :, 1984:2240], 256)
            if l == 1:
                load_cols(2304, w_in_vres[0].rearrange("(k p) n -> p k n", p=128), 32)
            wuq = sb(es, "wuq", [128, 2, 4, 256], BF16)
            wuq_st = sb(es, "wuq_st", [128, 2, 768])
            em.dma("sp", wuq_st[:, :, :], w_uq[l].rearrange("(k p) n -> p k n", p=128), w=[Bwst])
            for h in range(4):
                em.op("pool", lambda e, h=h: e.tensor_copy(wuq[:, :, h, 0:192], wuq_st[:, :, h * 192:(h + 1) * 192]), r=[Bwst], w=[Bw])
                em.op("pool", lambda e, h=h: e.tensor_copy(wuq[:, :, h, 192:224], wuq_st[:, :, h * 192 + 160:h * 192 + 192]), r=[Bwst], w=[Bw])
                em.op("pool", lambda e, h=h: e.tensor_copy(wuq[:, :, h, 224:256], wuq_st[:, :, h * 192 + 128:h * 192 + 160]), r=[Bwst], w=[Bw])
            wukv = sb(es, "wukv", [128, 1024], BF16)
            wukv_st = sb(es, "wukv_st", [128, 1024])
            em.dma("sp", wukv_st[:, :], w_ukv[l], w=[Bwst])
            for h in range(4):
                em.op("pool", lambda e, h=h: e.tensor_copy(wukv[:, h * 128:(h + 1) * 128], wukv_st[:, h * 256:h * 256 + 128]), r=[Bwst], w=[Bw])
                em.op("pool", lambda e, h=h: e.tensor_copy(wukv[:, 512 + h * 128:512 + (h + 1) * 128], wukv_st[:, h * 256 + 128:h * 256 + 256]), r=[Bwst], w=[Bw])
            def colvec(name, src_row, n):
                t = sb(es, name, [128, n])
                em.dma("sp", t[:, :], src_row.rearrange("(k p) -> p k", p=128), w=[Bw], allow_slow_non_contiguous=True)
                return t
            gq_col = colvec("gq_col", g_q_norm[l, :], 2)
            gkv_col = colvec("gkv_col", g_kv_norm[l, :], 1)
            mu_col = colvec("mu_col", mu_shift[l, :], 14)
            if l == 1:
                muv_col = sb(es, "muv_col", [32, 1]); omuv_col = sb(es, "omuv_col", [32, 1])
                em.dma("sp", muv_col[:, :], mu_shift_vres[0, :].rearrange("(p o) -> p o", o=1), w=[Bw])
                em.op("dve", lambda e: e.tensor_scalar(out=omuv_col[:, :], in0=muv_col[:, :], scalar1=-1.0, scalar2=1.0, op0=ALU.mult, op1=ALU.add), r=[Bw], w=[Bw])
            omu_col = sb(es, "omu_col", [128, 14])
            em.op("dve", lambda e: e.tensor_scalar(out=omu_col[:, :], in0=mu_col[:, :], scalar1=-1.0, scalar2=1.0, op0=ALU.mult, op1=ALU.add), r=[Bw], w=[Bw])

            xt = [sb(es, "xt%d" % i, [128, D]) for i in range(2)]; Bxt = [Buf(), Buf()]
            junk = sb(es, "junk", [128, D], BF16); ssum = sb(es, "ssum", [128, 1]); rstd = sb(es, "rstd", [128, 1])
            xn = sb(es, "xn", [128, D], BF16); Bnt = Buf()
            hT = sb(es, "hT", [128, 8, 512], BF16); BhT = Buf()
            pos_i = sb(es, "pos_i", [64, 512], I32); pos_f = sb(es, "pos_f", [64, 512])
            ang = sb(es, "ang", [64, 512]); cos2 = sb(es, "cos2", [64, 512]); sin2 = sb(es, "sin2", [64, 512]); Brope = Buf()
            mla_t = [sb(es, "mla_t%d" % i, [128, 512]) for i in range(6)]; Bm = [Buf() for _ in range(6)]
            cqn = sb(es, "cqn", [128, 2, 512], BF16); ckvn = sb(es, "ckvn", [128, 512], BF16); Bcn = Buf()
            stg_b = [sb(es, "stg_b%d" % i, [128, 512], BF16) for i in range(4)]; Bstg = [Buf() for _ in range(4)]
            stg_i = [0]
            qsq_row = sb(es, "qsq_row", [1, 512]); Bqs = Buf()
            praw = [sb(es, "praw%d" % i, [128, 513]) for i in range(2)]; Bpraw = [Buf(), Buf()]
            carry = sb(es, "carry", [128, 19]); Bcar = Buf()
            em.op("dve", lambda e: e.memset(carry[:, :], 0.0), w=[Bcar])
            rwst = [sb(es, "rwst%d" % i, [128, 512]) for i in range(2)]; Brwst = [Buf(), Buf()]
            rwtmp = sb(es, "rwtmp", [128, 512]); Brwtmp = Buf()

            def stg():
                i = stg_i[0]
                stg_i[0] = (i + 1) % 4
                return stg_b[i], Bstg[i]

            for g in range(NG):
                t0 = g * 512
                for tt in range(4):
                    xi = (g * 4 + tt) % 2
                    em.dma("sp", xt[xi][:, :], x_src[t0 + tt * 128:t0 + (tt + 1) * 128, :], r=[Bxsrc], w=[Bxt[xi]])
                    rmsnorm_T(es, xt[xi], Bxt[xi], gs1[l], sh1_col, hT, BhT, tt * 128, (junk, ssum, rstd, xn, Bnt))
                em.dma("sp", pos_i[:, :], pos_in[0:1, t0:t0 + 512].to_broadcast([64, 512]), w=[Brope])
                em.op("dve", lambda e: e.tensor_copy(pos_f[:, :], pos_i[:, :]), r=[Brope], w=[Brope])
                em.op("dve", lambda e: e.tensor_scalar(out=ang[:, :], in0=pos_f[:, :], scalar1=invf[:, 0:1], scalar2=None, op0=ALU.mult), r=[Brope, Bc], w=[Brope])
                def sintab(dst, off):
                    em.op("dve", lambda e: e.tensor_scalar(out=pos_f[:, :], in0=ang[:, :], scalar1=1.0 / (2 * math.pi), scalar2=off, op0=ALU.mult, op1=ALU.add), r=[Brope], w=[Brope])
                    em.op("dve", lambda e: e.tensor_copy(pos_i[:, :], pos_f[:, :]), r=[Brope], w=[Brope])
                    em.op("dve", lambda e: e.tensor_copy(dst[:, :], pos_i[:, :]), r=[Brope], w=[Brope])
                    em.op("dve", lambda e: e.tensor_tensor(out=pos_f[:, :], in0=pos_f[:, :], in1=dst[:, :], op=ALU.subtract), r=[Brope], w=[Brope])
                    em.op("dve", lambda e: e.tensor_scalar(out=dst[:, :], in0=pos_f[:, :], scalar1=0.5, scalar2=None, op0=ALU.is_gt), r=[Brope], w=[Brope])
                    em.op("dve", lambda e: e.tensor_tensor(out=pos_f[:, :], in0=pos_f[:, :], in1=dst[:, :], op=ALU.subtract), r=[Brope], w=[Brope])
                    em.op("dve", lambda e: e.tensor_scalar(out=dst[:, :], in0=pos_f[:, :], scalar1=-0.5, scalar2=None, op0=ALU.is_lt), r=[Brope], w=[Brope])
                    em.op("dve", lambda e: e.tensor_tensor(out=pos_f[:, :], in0=pos_f[:, :], in1=dst[:, :], op=ALU.add), r=[Brope], w=[Brope])
                    em.op("act", lambda e: e.activation(out=dst[:, :], in_=pos_f[:, :], func=AF.Sin, scale=2 * math.pi), r=[Brope], w=[Brope])
                sintab(cos2, 0.25)
                sintab(sin2, 0.0)
                em.op("dve", lambda e: e.tensor_scalar(out=sin2[:, :], in0=sin2[:, :], scalar1=sgn[:, 0:1], scalar2=None, op0=ALU.mult), r=[Brope, Bc], w=[Brope])

                def proj_chunk(c0, m, use_rows=128):
                    pt, bp = nps()
                    for k in range(8):
                        em.op("pe", lambda e, k=k: e.matmul(pt[0:m, :], lhsT=wcat[:, k, c0:c0 + m], rhs=hT[:, k, :], start=(k == 0), stop=(k == 7)), r=[Bw, BhT], w=[bp])
                    return pt, bp

                cq_f = [mla_t[0], mla_t[1]]
                for j in range(2):
                    pt, bp = proj_chunk(j * 128, 128)
                    em.op("act", lambda e, j=j, pt=pt: e.activation(out=cq_f[j][:, :], in_=pt[:, :], func=AF.Copy), r=[bp], w=[Bm[j]])
                    em.op("dve", lambda e, j=j: e.tensor_tensor(out=mla_t[2 + j][:, :], in0=cq_f[j][:, :], in1=cq_f[j][:, :], op=ALU.mult), r=[Bm[j]], w=[Bm[2 + j]])
                pss, bpss = nps()
                for j in range(2):
                    em.op("pe", lambda e, j=j: e.matmul(pss[:, :], lhsT=ones_f[:, :], rhs=mla_t[2 + j][:, :], start=(j == 0), stop=(j == 1)), r=[Bc, Bm[2 + j]], w=[bpss])
                rstd_op(mla_t[4][:, :], pss[:, :], 1.0 / 256, [bpss], [Bm[4]])
                for j in range(2):
                    em.op("dve", lambda e, j=j: e.scalar_tensor_tensor(out=cqn[:, j, :], in0=cq_f[j][:, :], scalar=gq_col[:, j:j + 1], in1=mla_t[4][:, :], op0=ALU.mult, op1=ALU.mult), r=[Bm[j], Bm[4], Bw], w=[Bcn])
                pt, bp = proj_chunk(256, 128)
                em.op("act", lambda e, pt=pt: e.activation(out=mla_t[0][:, :], in_=pt[:, :], func=AF.Copy), r=[bp], w=[Bm[0]])
                em.op("dve", lambda e: e.tensor_tensor(out=mla_t[2][:, :], in0=mla_t[0][:, :], in1=mla_t[0][:, :], op=ALU.mult), r=[Bm[0]], w=[Bm[2]])
                pss, bpss = nps()
                em.op("pe", lambda e: e.matmul(pss[:, :], lhsT=ones_f[:, :], rhs=mla_t[2][:, :], start=True, stop=True), r=[Bc, Bm[2]], w=[bpss])
                rstd_op(mla_t[4][:, :], pss[:, :], 1.0 / 128, [bpss], [Bm[4]])
                em.op("dve", lambda e: e.scalar_tensor_tensor(out=ckvn[:, :], in0=mla_t[0][:, :], scalar=gkv_col[:, 0:1], in1=mla_t[4][:, :], op0=ALU.mult, op1=ALU.mult), r=[Bm[0], Bm[4], Bw], w=[Bcn])
                pk, bpk = proj_chunk(384, 64)
                pks, bpks = proj_chunk(448, 64)
                em.op("dve", lambda e: e.tensor_tensor(out=mla_t[0][0:64, :], in0=pk[0:64, :], in1=cos2[:, :], op=ALU.mult), r=[bpk, Brope], w=[Bm[0]])
                em.op("dve", lambda e: e.tensor_tensor(out=mla_t[1][0:64, :], in0=pks[0:64, :], in1=sin2[:, :], op=ALU.mult), r=[bpks, Brope], w=[Bm[1]])
                sg, bsg = stg()
                em.op("dve", lambda e: e.tensor_tensor(out=sg[0:64, :], in0=mla_t[0][0:64, :], in1=mla_t[1][0:64, :], op=ALU.add), r=[Bm[0], Bm[1]], w=[bsg])
                em.dma("sp", kpe_d[:, t0:t0 + 512], sg[0:64, :], r=[bsg], w=[B_k])
                for h in range(4):
                    pt, bp = nps()
                    em.op("pe", lambda e, h=h, pt=pt: e.matmul(pt[:, :], lhsT=wukv[:, h * 128:(h + 1) * 128], rhs=ckvn[:, :], start=True, stop=True), r=[Bw, Bcn], w=[bp])
                    sg, bsg = stg()
                    em.op("act", lambda e, pt=pt, sg=sg: e.activation(out=sg[:, :], in_=pt[:, :], func=AF.Copy), r=[bp], w=[bsg])
                    em.dma("sp", kn_d[h, :, t0:t0 + 512], sg[:, :], r=[bsg], w=[B_k])
                for tt in range(4):
                    pt, bp = nps()
                    em.op("pe", lambda e, tt=tt, pt=pt: e.matmul(pt[:, :], lhsT=ckvn[:, tt * 128:(tt + 1) * 128], rhs=wukv[:, 512:1024], start=True, stop=True), r=[Bw, Bcn], w=[bp])
                    sg, bsg = stg()
                    em.op("act", lambda e, pt=pt, sg=sg: e.activation(out=sg[:, :], in_=pt[:, :], func=AF.Copy), r=[bp], w=[bsg])
                    em.dma("sp", v_d[t0 + tt * 128:t0 + (tt + 1) * 128, :], sg[:, :], r=[bsg], w=[B_k])
                for h in range(4):
                    pq, bpq = nps()
                    for k in range(2):
                        em.op("pe", lambda e, k=k, h=h, pq=pq: e.matmul(pq[:, :], lhsT=wuq[:, k, h, 0:128], rhs=cqn[:, k, :], start=(k == 0), stop=(k == 1)), r=[Bw, Bcn], w=[bpq])
                    sg, bsg = stg()
                    em.op("act", lambda e, pq=pq, sg=sg: e.activation(out=sg[:, :], in_=pq[:, :], func=AF.Copy), r=[bpq], w=[bsg])
                    em.dma("sp", qn_d[h, :, t0:t0 + 512], sg[:, :], r=[bsg], w=[B_q])
                    em.op("dve", lambda e, pq=pq: e.tensor_tensor(out=mla_t[2][:, :], in0=sg[:, :], in1=sg[:, :], op=ALU.mult), r=[bsg], w=[Bm[2]])
                    pp, bpp = nps()
                    pp2, bpp2 = nps()
                    for k in range(2):
                        em.op("pe", lambda e, k=k, h=h, pp=pp: e.matmul(pp[0:64, :], lhsT=wuq[:, k, h, 128:192], rhs=cqn[:, k, :], start=(k == 0), stop=(k == 1)), r=[Bw, Bcn], w=[bpp])
                    for k in range(2):
                        em.op("pe", lambda e, k=k, h=h, pp2=pp2: e.matmul(pp2[0:64, :], lhsT=wuq[:, k, h, 192:256], rhs=cqn[:, k, :], start=(k == 0), stop=(k == 1)), r=[Bw, Bcn], w=[bpp2])
                    em.op("dve", lambda e, pp=pp: e.tensor_tensor(out=mla_t[0][0:64, :], in0=pp[0:64, :], in1=cos2[:, :], op=ALU.mult), r=[bpp, Brope], w=[Bm[0]])
                    em.op("dve", lambda e, pp2=pp2: e.tensor_tensor(out=mla_t[1][0:64, :], in0=pp2[0:64, :], in1=sin2[:, :], op=ALU.mult), r=[bpp2, Brope], w=[Bm[1]])
                    sg2, bsg2 = stg()
                    em.op("dve", lambda e, sg2=sg2: e.tensor_tensor(out=sg2[0:64, :], in0=mla_t[0][0:64, :], in1=mla_t[1][0:64, :], op=ALU.add), r=[Bm[0], Bm[1]], w=[bsg2])
                    em.dma("sp", qpe_d[h, :, t0:t0 + 512], sg2[0:64, :], r=[bsg2], w=[B_q])
                    em.op("dve", lambda e, sg2=sg2: e.tensor_tensor(out=mla_t[3][0:64, :], in0=sg2[0:64, :], in1=sg2[0:64, :], op=ALU.mult), r=[bsg2], w=[Bm[3]])
                    pr, bpr = nps()
                    em.op("pe", lambda e, pr=pr: e.matmul(pr[0:1, :], lhsT=ones_f[:, 0:1], rhs=mla_t[2][:, :], start=True, stop=False), r=[Bc, Bm[2]], w=[bpr])
                    em.op("pe", lambda e, pr=pr: e.matmul(pr[0:1, :], lhsT=ones_f[0:64, 0:1], rhs=mla_t[3][0:64, :], start=False, stop=True), r=[Bc, Bm[3]], w=[bpr])
                    em.op("act", lambda e, pr=pr: e.activation(out=qsq_row[:, :], in_=pr[0:1, :], func=AF.Copy), r=[bpr], w=[Bqs])
                    em.dma("sp", qsq_d[h:h + 1, t0:t0 + 512], qsq_row[:, :], r=[Bqs], w=[B_q])

                if dbg and dbg[0] == "mla_prep":
                    continue
                nrw = 14 if l == 0 else 15
                for ci in range(nrw):
                    rows = 128 if ci < 14 else 32
                    c0 = 512 + ci * 128 if ci < 14 else 2304
                    pt, bp = proj_chunk(c0, rows)
                    pi_ = ci % 2
                    pr_, bpr_ = praw[pi_], Bpraw[pi_]
                    em.op("act", lambda e, pt=pt, pr_=pr_, rows=rows: e.activation(out=pr_[0:rows, 1:513], in_=pt[0:rows, :], func=AF.Copy), r=[bp], w=[bpr_])
                    em.op("dve", lambda e, pr_=pr_, rows=rows, ci=ci: e.tensor_copy(pr_[0:rows, 0:1], carry[0:rows, ci:ci + 1]), r=[Bcar], w=[bpr_])
                    mu_ap = mu_col[:, ci:ci + 1] if ci < 14 else muv_col[:, 0:1]
                    omu_ap = omu_col[:, ci:ci + 1] if ci < 14 else omuv_col[:, 0:1]
                    em.op("dve", lambda e, pr_=pr_, rows=rows, omu_ap=omu_ap: e.tensor_scalar(out=rwtmp[0:rows, :], in0=pr_[0:rows, 1:513], scalar1=omu_ap, scalar2=None, op0=ALU.mult), r=[bpr_, Bw], w=[Brwtmp])
                    ri = ci % 2
                    em.op("dve", lambda e, pr_=pr_, rows=rows, mu_ap=mu_ap, ri=ri: e.scalar_tensor_tensor(out=rwst[ri][0:rows, :], in0=pr_[0:rows, 0:512], scalar=mu_ap, in1=rwtmp[0:rows, :], op0=ALU.mult, op1=ALU.add), r=[bpr_, Brwtmp, Bw], w=[Brwst[ri]])
                    em.op("dve", lambda e, pr_=pr_, rows=rows, ci=ci: e.tensor_copy(carry[0:rows, ci:ci + 1], pr_[0:rows, 512:513]), r=[bpr_], w=[Bcar])
                    em.dma("sp", rw_d[ci, 0:rows, t0:t0 + 512], rwst[ri][0:rows, :], r=[Brwst[ri]], w=[B_rw])
            em.barrier()
        if dbg and dbg[0] == "mla_prep":
            break
        with ExitStack() as es:
            Bv = Buf()

            def colvec2(name, src_row, n):
                t = sb(es, name, [128, n])
                em.dma("sp", t[:, :], src_row.rearrange("(k p) -> p k", p=128), w=[Bv], allow_slow_non_contiguous=True)
                return t
            w0_col = colvec2("w0_col", w0_in[l, :], 4)
            a0_col = colvec2("a0_col", a0_in[l, :], 4)
            kk_col = colvec2("kk_col", k_k_in[l, :], 4)
            ka_col = colvec2("ka_col", k_a_in[l, :], 4)
            rk_col = colvec2("rk_col", r_k_in[l, :], 4)
            lora_w = sb(es, "lora_w", [128, 2, 512])
            lora_a2 = sb(es, "lora_a2", [64, 512]); lora_ain = sb(es, "lora_ain", [64, 512])
            em.dma("sp", lora_w[0:64, 0, :], w2_in[l], w=[Bv])
            em.dma("sp", lora_a2[:, :], a2_in[l], w=[Bv])
            em.dma("sp", lora_w[:, 1, :], g2_in[l], w=[Bv])
            lnw_bc = sb(es, "lnw_bc", [128, 512]); lnb_bc = sb(es, "lnb_bc", [128, 512])
            em.dma("sp", lnw_bc[:, :], ln_w_in[l:l + 1, :].to_broadcast([128, 512]), w=[Bv])
            em.dma("sp", lnb_bc[:, :], ln_b_in[l:l + 1, :].to_broadcast([128, 512]), w=[Bv])
            if l == 1:
                v0_col = colvec2("v0_col", v0_in[0, :], 4)
                v2_sb = sb(es, "v2_sb", [32, 512])
                em.dma("sp", v2_sb[:, :], v2_in[0], w=[Bv])
                vl = sb(es, "vl", [32, 512])
            big = {}
            for nm in ("R", "K", "V", "LW", "CA", "CB", "AS", "T1", "T2", "T3", "T4", "T5"):
                big[nm] = (sb(es, "big_" + nm, [128, 4, 512]), Buf())
            lorwa = sb(es, "lorwa", [128, 512]); lorg = sb(es, "lorg", [128, 512]); tw = sb(es, "tw", [64, 512]); Blo = Buf()
            Sst = sb(es, "Sst", [128, 4, 64]); BS = Buf()
            em.op("dve", lambda e: e.memset(Sst[:, :, :], 0.0), w=[BS])
            pcs = sb(es, "pcs", [128, 16]); Bpc = Buf()
            mats = {}
            for nm in ("N", "M", "AkT", "GbT", "GkT", "Tt", "X2", "XT2"):
                mats[nm] = (sb(es, "mat_" + nm, [128, 8, 128], F32), [Buf(), Buf()])
            tokt = {}
            for nm in ("Vtok", "Bhtok", "Khtok", "Wsb", "Usb", "Ysb", "cen", "sq", "gte"):
                tokt[nm] = (sb(es, "tok_" + nm, [128, 512], F32), Buf())
            bonus = sb(es, "bonus", [128, 32]); Bbon = Buf()
            st8 = [sb(es, "st8_%d" % i, [128, 8]) for i in range(3)]; Bst8 = Buf()
            outb = sb(es, "outb", [128, 512], BF16); Boutb = Buf()
            yTb = sb(es, "yTb", [128, 4, 128], BF16); ByTb = Buf()

            def v3(t):
                return t[:, :, :].rearrange("p c (n t) -> p (c n) t", t=128)

            def fl(t):
                return t[:, :, :].rearrange("p c t -> p (c t)")
            (R_, BR), (K_, BK_), (V_, BV_), (LW, BLW), (CA, BCA), (CB, BCB) = (big[n] for n in ("R", "K", "V", "LW", "CA", "CB"))
            (AS, BAS), (T1, BT1), (T2, BT2), (T3, BT3), (T4, BT4), (T5, BT5) = (big[n] for n in ("AS", "T1", "T2", "T3", "T4", "T5"))

            rstop = dbg[3] if (dbg and len(dbg) > 3) else 99
            for g in range(NG):
                if rstop < 99 and g > 0:
                    continue
                t0 = g * 512
                for cc in range(4):
                    em.dma("sp", R_[:, cc, :], rw_d[cc, :, t0:t0 + 512], r=[B_rw], w=[BR])
                    em.dma("sp", K_[:, cc, :], rw_d[4 + cc, :, t0:t0 + 512], r=[B_rw], w=[BK_])
                    em.dma("sp", V_[:, cc, :], rw_d[8 + cc, :, t0:t0 + 512], r=[B_rw], w=[BV_])
                em.dma("sp", lorwa[:, :], rw_d[12, :, t0:t0 + 512], r=[B_rw], w=[Blo])
                em.dma("sp", lorg[:, :], rw_d[13, :, t0:t0 + 512], r=[B_rw], w=[Blo])
                em.dma("sp", lora_ain[:, :], rw_d[12, 64:128, t0:t0 + 512], r=[B_rw], w=[Blo])
                em.op("act", lambda e: e.activation(out=tw[:, :], in_=lorwa[0:64, :], func=AF.Tanh), r=[Blo], w=[Blo])
                for cc in range(4):
                    pw, bpw = nps()
                    em.op("pe", lambda e, cc=cc, pw=pw: e.matmul(pw[:, :], lhsT=lora_w[0:64, 0, cc * 128:(cc + 1) * 128], rhs=tw[0:64, :], start=True, stop=True), r=[Bv, Blo], w=[bpw])
                    em.op("act", lambda e, cc=cc, pw=pw: e.activation(out=LW[:, cc, :], in_=pw[:, :], func=AF.Sigmoid, bias=w0_col[:, cc:cc + 1]), r=[bpw, Bv], w=[BLW])
                    pa, bpa = nps()
                    em.op("pe", lambda e, cc=cc, pa=pa: e.matmul(pa[:, :], lhsT=lora_a2[0:64, cc * 128:(cc + 1) * 128], rhs=lora_ain[0:64, :], start=True, stop=True), r=[Bv, Blo], w=[bpa])
                    em.op("act", lambda e, cc=cc, pa=pa: e.activation(out=AS[:, cc, :], in_=pa[:, :], func=AF.Sigmoid, bias=a0_col[:, cc:cc + 1]), r=[bpa, Bv], w=[BAS])
                em.op("dve", lambda e: e.tensor_scalar(out=fl(LW), in0=fl(LW), scalar1=-DEC_C, scalar2=None, op0=ALU.mult), r=[BLW], w=[BLW])
                if l == 0:
                    for cc in range(4):
                        em.dma("sp", vfirst_d[cc, :, t0:t0 + 512], V_[:, cc, :], r=[BV_], w=[B_vf])
                else:
                    em.dma("sp", vl[:, :], rw_d[14, 0:32, t0:t0 + 512], r=[B_rw], w=[Blo])
                    for cc in range(4):
                        pv, bpv = nps()
                        em.op("pe", lambda e, cc=cc, pv=pv: e.matmul(pv[:, :], lhsT=v2_sb[0:32, cc * 128:(cc + 1) * 128], rhs=vl[0:32, :], start=True, stop=True), r=[Bv, Blo], w=[bpv])
                        em.op("act", lambda e, cc=cc, pv=pv: e.activation(out=T1[:, cc, :], in_=pv[:, :], func=AF.Sigmoid, bias=v0_col[:, cc:cc + 1]), r=[bpv, Bv], w=[BT1])
                        em.dma("sp", T2[:, cc, :], vfirst_d[cc, :, t0:t0 + 512], r=[B_vf], w=[BT2])
                    em.op("dve", lambda e: e.tensor_tensor(out=fl(T2), in0=fl(T2), in1=fl(V_), op=ALU.subtract), r=[BT2, BV_], w=[BT2])
                    em.op("dve", lambda e: e.tensor_tensor(out=fl(T2), in0=fl(T2), in1=fl(T1), op=ALU.mult), r=[BT2, BT1], w=[BT2])
                    em.op("dve", lambda e: e.tensor_tensor(out=fl(V_), in0=fl(V_), in1=fl(T2), op=ALU.add), r=[BT2, BV_], w=[BV_])
                for cc in range(4):
                    em.op("dve", lambda e, cc=cc: e.tensor_scalar(out=T1[:, cc, :], in0=K_[:, cc, :], scalar1=kk_col[:, cc:cc + 1], scalar2=None, op0=ALU.mult), r=[BK_, Bv], w=[BT1])
                em.op("dve", lambda e: e.tensor_tensor(out=fl(T2), in0=fl(T1), in1=fl(T1), op=ALU.mult), r=[BT1], w=[BT2])
                for cc in range(4):
                    pss, bpss = nps()
                    em.op("pe", lambda e, cc=cc, pss=pss: e.matmul(pss[:, :], lhsT=blk_f[:, :], rhs=T2[:, cc, :], start=True, stop=True), r=[Bc, BT2], w=[bpss])
                    em.op("act", lambda e, cc=cc, pss=pss: e.activation(out=T3[:, cc, :], in_=pss[:, :], func=AF.Sqrt), r=[bpss], w=[BT3])
                em.op("dve", lambda e: e.tensor_scalar(out=fl(T3), in0=fl(T3), scalar1=1e-12, scalar2=None, op0=ALU.max), r=[BT3], w=[BT3])
                em.op("dve", lambda e: e.reciprocal(out=fl(T3), in_=fl(T3)), r=[BT3], w=[BT3])
                em.op("dve", lambda e: e.tensor_tensor(out=fl(T1), in0=fl(T1), in1=fl(T3), op=ALU.mult), r=[BT1, BT3], w=[BT1])
                for cc in range(4):
                    em.op("dve", lambda e, cc=cc: e.tensor_scalar(out=T2[:, cc, :], in0=AS[:, cc, :], scalar1=-1.0, scalar2=ka_col[:, cc:cc + 1], op0=ALU.add, op1=ALU.mult), r=[BAS, Bv], w=[BT2])
                em.op("dve", lambda e: e.scalar_tensor_tensor(out=fl(K_), in0=fl(T2), scalar=1.0, in1=fl(K_), op0=ALU.add, op1=ALU.mult), r=[BT2, BK_], w=[BK_])
                for cc in range(4):
                    em.op("dve", lambda e, cc=cc: e.scalar_tensor_tensor(out=T3[:, cc, :], in0=R_[:, cc, :], scalar=rk_col[:, cc:cc + 1], in1=K_[:, cc, :], op0=ALU.mult, op1=ALU.mult), r=[BR, BK_, Bv], w=[BT3])
                pb, bpb = nps()
                for n in range(4):
                    for cc in range(4):
                        em.op("pe", lambda e, n=n, cc=cc: e.matmul(pb[:, n * 8 + cc * 2:n * 8 + cc * 2 + 2], lhsT=T3[:, cc, n * 128:(n + 1) * 128], rhs=hsel[:, :], start=True, stop=True), r=[BT3, Bc], w=[bpb])
                em.op("act", lambda e: e.activation(out=bonus[:, :], in_=pb[:, 0:32], func=AF.Copy), r=[bpb], w=[Bbon])
                em.op("dve", lambda e: e.tensor_tensor(out=fl(T2), in0=fl(T1), in1=fl(AS), op=ALU.mult), r=[BT1, BAS], w=[BT2])
                em.op("dve", lambda e: e.tensor_scalar(out=fl(T1), in0=fl(T1), scalar1=-1.0, scalar2=None, op0=ALU.mult), r=[BT1], w=[BT1])
                src, bsrc = LW, BLW
                for si, sft in enumerate((1, 2, 4, 8, 16, 32, 64)):
                    dst, bdst = (CA, BCA) if si % 2 == 0 else (CB, BCB)
                    em.op("act", lambda e, src=src, dst=dst, sft=sft: e.activation(out=v3(dst)[:, :, 0:sft], in_=v3(src)[:, :, 0:sft], func=AF.Copy), r=[bsrc], w=[bdst])
                    em.op("dve", lambda e, src=src, dst=dst, sft=sft: e.tensor_tensor(out=v3(dst)[:, :, sft:128], in0=v3(src)[:, :, sft:128], in1=v3(src)[:, :, 0:128 - sft], op=ALU.add), r=[bsrc], w=[bdst])
                    src, bsrc = dst, bdst
                assert src is CA
                em.op("act", lambda e: e.activation(out=fl(T3), in_=fl(CA), func=AF.Exp), r=[BCA], w=[BT3])
                em.op("dve", lambda e: e.tensor_tensor(out=fl(R_), in0=fl(R_), in1=fl(T3), op=ALU.mult), r=[BR, BT3], w=[BR])
                em.op("dve", lambda e: e.tensor_tensor(out=fl(CB), in0=fl(CA), in1=fl(LW), op=ALU.subtract), r=[BCA, BLW], w=[BCB])
                em.op("act", lambda e: e.activation(out=fl(T3), in_=fl(CB), func=AF.Exp), r=[BCB], w=[BT3])
                em.op("dve", lambda e: e.tensor_tensor(out=fl(T1), in0=fl(T1), in1=fl(T3), op=ALU.mult), r=[BT1, BT3], w=[BT1])
                em.op("act", lambda e: e.activation(out=fl(T3), in_=fl(CA), func=AF.Exp, scale=-1.0), r=[BCA], w=[BT3])
                em.op("dve", lambda e: e.tensor_tensor(out=fl(T4), in0=fl(T2), in1=fl(T3), op=ALU.mult), r=[BT2, BT3], w=[BT4])
                em.op("dve", lambda e: e.tensor_tensor(out=fl(T5), in0=fl(K_), in1=fl(T3), op=ALU.mult), r=[BK_, BT3], w=[BT5])
                em.op("dve", lambda e: e.tensor_tensor(out=v3(CB), in0=v3(CA)[:, :, 127:128].to_broadcast([128, 16, 128]), in1=v3(CA), op=ALU.subtract), r=[BCA], w=[BCB])
                em.op("act", lambda e: e.activation(out=fl(T3), in_=fl(CB), func=AF.Exp), r=[BCB], w=[BT3])
                em.op("dve", lambda e: e.tensor_tensor(out=fl(T2), in0=fl(T2), in1=fl(T3), op=ALU.mult), r=[BT2, BT3], w=[BT2])
                em.op("dve", lambda e: e.tensor_tensor(out=fl(K_), in0=fl(K_), in1=fl(T3), op=ALU.mult), r=[BK_, BT3], w=[BK_])
                em.op("act", lambda e: e.activation(out=pcs[:, :].unsqueeze(2), in_=v3(CA)[:, :, 127:128], func=AF.Exp), r=[BCA], w=[Bpc])
                em.op("dve", lambda e: e.tensor_scalar(out=fl(CB), in0=fl(T1), scalar1=hsel[:, 1:2], scalar2=None, op0=ALU.mult), r=[BT1, Bc, BCA], w=[BCB])
                em.op("dve", lambda e: e.tensor_scalar(out=fl(CA), in0=fl(T1), scalar1=hsel[:, 0:1], scalar2=None, op0=ALU.mult), r=[BT1, Bc, Bpc], w=[BCA])
                em.op("dve", lambda e: e.tensor_scalar(out=fl(LW), in0=fl(R_), scalar1=hsel[:, 0:1], scalar2=None, op0=ALU.mult), r=[BR, Bc], w=[BLW])
                em.op("dve", lambda e: e.tensor_scalar(out=fl(AS), in0=fl(R_), scalar1=hsel[:, 1:2], scalar2=None, op0=ALU.mult), r=[BR, Bc], w=[BAS])
                Am = ((CA, BCA), (CB, BCB)); Rm = ((LW, BLW), (AS, BAS))
                em.op("act", lambda e: e.activation(out=lorg[:, :], in_=lorg[:, :], func=AF.Sigmoid), r=[Blo], w=[Blo])

                if rstop <= 1:
                    continue
                for n in range(4):
                    if rstop < 99 and n > 0:
                        continue
                    ts_ = slice(n * 128, (n + 1) * 128)

                    def hs(t, h):
                        return t[(h % 2) * 64:(h % 2) * 64 + 64, h // 2, ts_]
                    import os as _os

                    def full(t, h):
                        return t[:, h // 2, ts_]
                    specs = (("N", "b", "A", mstrict), ("M", "A", "b", mstrictL), ("AkT", "k", "A", mstrict), ("GbT", "b", "R", mincl), ("GkT", "k", "R", mincl))

                    def opnd(code, h):
                        if code == "b":
                            return full(T4, h), BT4
                        if code == "k":
                            return full(T5, h), BT5
                        if code == "A":
                            return full(Am[h % 2][0], h), Am[h % 2][1]
                        return full(Rm[h % 2][0], h), Rm[h % 2][1]
                    for (nm, Lc, Rc, msk) in specs:
                        mt, bm = mats[nm]
                        for hb in range(2):
                            pm, bpm = nps()
                            for h4 in range(4):
                                h = hb * 4 + h4
                                La, Lb = opnd(Lc, h)
                                Ra, Rb = opnd(Rc, h)
                                em.op("pe", lambda e, h4=h4, pm=pm, La=La, Ra=Ra: e.matmul(pm[:, h4 * 128:(h4 + 1) * 128], lhsT=La, rhs=Ra, start=True, stop=True), r=[Lb, Rb], w=[bpm])
                            em.op("dve", lambda e, pm=pm, mt=mt, hb=hb, msk=msk: e.tensor_tensor(out=mt[:, hb * 4:(hb + 1) * 4, :], in0=pm[:, :].rearrange("p (a b) -> p a b", b=128), in1=msk[:, :].unsqueeze(1).to_broadcast([128, 4, 128]), op=ALU.mult), r=[bpm, Bc], w=[bm[hb]])
                    em.log = False
                    if rstop <= 2:
                        continue
                    Tt, bTt = mats["Tt"]
                    for hb in range(2):
                        em.op("dve", lambda e, hb=hb: e.tensor_tensor(out=Tt[:, hb * 4:(hb + 1) * 4, :], in0=mats["N"][0][:, hb * 4:(hb + 1) * 4, :], in1=ident_f[:, :].unsqueeze(1).to_broadcast([128, 4, 128]), op=ALU.add), r=[mats["N"][1][hb], Bc], w=[bTt[hb]])
                    Xc, XTc, Xn, XTn = "N", "M", "X2", "XT2"
                    for step in range(6):
                        lastst = (step == 5)
                        for hb in range(2):
                            if not lastst:
                                pX, bpX = nps()
                                for h4 in range(4):
                                    h = hb * 4 + h4
                                    em.op("pe", lambda e, h=h, h4=h4, pX=pX, Xc=Xc, XTc=XTc: e.matmul(pX[:, h4 * 128:(h4 + 1) * 128], lhsT=mats[XTc][0][:, h, :], rhs=mats[Xc][0][:, h, :], start=True, stop=True), r=[mats[XTc][1][hb], mats[Xc][1][hb]], w=[bpX])
                            pXT, bpXT = nps()
                            for h4 in range(4):
                                h = hb * 4 + h4
                                em.op("pe", lambda e, h=h, h4=h4, pXT=pXT, Xc=Xc, XTc=XTc: e.matmul(pXT[:, h4 * 128:(h4 + 1) * 128], lhsT=mats[Xc][0][:, h, :], rhs=mats[XTc][0][:, h, :], start=True, stop=True), r=[mats[XTc][1][hb], mats[Xc][1][hb]], w=[bpXT])
                            if not lastst:
                                em.op("act", lambda e, pX=pX, Xn=Xn, hb=hb: e.activation(out=mats[Xn][0][:, hb * 4:(hb + 1) * 4, :], in_=pX[:, :].rearrange("p (a b) -> p a b", b=128), func=AF.Copy), r=[bpX], w=[mats[Xn][1][hb]])
                            em.op("dve", lambda e, pXT=pXT, XTn=XTn, hb=hb: e.tensor_copy(mats[XTn][0][:, hb * 4:(hb + 1) * 4, :], pXT[:, :].rearrange("p (a b) -> p a b", b=128)), r=[bpXT], w=[mats[XTn][1][hb]])
                        for hb in range(2):
                            pT, bpT = nps()
                            for h4 in range(4):
                                h = hb * 4 + h4
                                em.op("pe", lambda e, h=h, h4=h4, pT=pT, XTn=XTn: e.matmul(pT[:, h4 * 128:(h4 + 1) * 128], lhsT=mats[XTn][0][:, h, :], rhs=Tt[:, h, :], start=True, stop=True), r=[mats[XTn][1][hb], bTt[hb]], w=[bpT])
                            em.op("dve", lambda e, pT=pT, hb=hb: e.tensor_tensor(out=Tt[:, hb * 4:(hb + 1) * 4, :], in0=pT[:, :].rearrange("p (a b) -> p a b", b=128), in1=Tt[:, hb * 4:(hb + 1) * 4, :], op=ALU.add), r=[bpT, bTt[hb]], w=[bTt[hb]])
                        Xc, XTc, Xn, XTn = Xn, XTn, Xc, XTc
                    if rstop <= 3:
                        continue
                    for (nm, src_t, bsrc_t) in (("Vtok", V_, BV_), ("Bhtok", T2, BT2), ("Khtok", K_, BK_)):
                        ptt, bptt = nps()
                        for cc in range(4):
                            em.op("pe", lambda e, cc=cc, ptt=ptt, src_t=src_t: e.transpose(ptt[:, cc * 128:(cc + 1) * 128], src_t[:, cc, ts_], ident_f[:, :]), r=[bsrc_t, Bc], w=[bptt])
                        em.op("act", lambda e, ptt=ptt, nm=nm: e.activation(out=tokt[nm][0][:, :], in_=ptt[:, :], func=AF.Copy), r=[bptt], w=[tokt[nm][1]])
                    Vtok, BVt = tokt["Vtok"]; Bhtok, BBh = tokt["Bhtok"]; Khtok, BKh = tokt["Khtok"]
                    Wsb, BW_ = tokt["Wsb"]; Usb, BU_ = tokt["Usb"]; Ysb, BY_ = tokt["Ysb"]
                    AkT, bAk = mats["AkT"]; GbT, bGb = mats["GbT"]; GkT, bGk = mats["GkT"]

                    def sst(h):
                        return Sst[:, h // 2, :]
                    if rstop <= 4:
                        continue
                    pW, bpW = nps()
                    for h in range(8):
                        em.op("pe", lambda e, h=h: e.matmul(pW[:, h * 64:(h + 1) * 64], lhsT=full(Am[h % 2][0], h), rhs=sst(h), start=True, stop=False), r=[Am[h % 2][1], BS], w=[bpW])
                        em.op("pe", lambda e, h=h: e.matmul(pW[:, h * 64:(h + 1) * 64], lhsT=AkT[:, h, :], rhs=Vtok[:, h * 64:(h + 1) * 64], start=False, stop=True), r=[bAk[h // 4], BVt], w=[bpW])
                    em.op("act", lambda e: e.activation(out=Wsb[:, :], in_=pW[:, :], func=AF.Copy), r=[bpW], w=[BW_])
                    pU, bpU = nps()
                    for h in range(8):
                        em.op("pe", lambda e, h=h: e.matmul(pU[:, h * 64:(h + 1) * 64], lhsT=Tt[:, h, :], rhs=Wsb[:, h * 64:(h + 1) * 64], start=True, stop=True), r=[bTt[h // 4], BW_], w=[bpU])
                    em.op("dve", lambda e: e.tensor_copy(Usb[:, :], pU[:, :]), r=[bpU], w=[BU_])
                    pY, bpY = nps()
                    for h in range(8):
                        em.op("pe", lambda e, h=h: e.matmul(pY[:, h * 64:(h + 1) * 64], lhsT=full(Rm[h % 2][0], h), rhs=sst(h), start=True, stop=False), r=[Rm[h % 2][1], BS], w=[bpY])
                        em.op("pe", lambda e, h=h: e.matmul(pY[:, h * 64:(h + 1) * 64], lhsT=GbT[:, h, :], rhs=Usb[:, h * 64:(h + 1) * 64], start=False, stop=False), r=[bGb[h // 4], BU_], w=[bpY])
                        em.op("pe", lambda e, h=h: e.matmul(pY[:, h * 64:(h + 1) * 64], lhsT=GkT[:, h, :], rhs=Vtok[:, h * 64:(h + 1) * 64], start=False, stop=True), r=[bGk[h // 4], BVt], w=[bpY])
                    em.op("act", lambda e: e.activation(out=Ysb[:, :], in_=pY[:, :], func=AF.Copy), r=[bpY], w=[BY_])
                    if rstop <= 5:
                        continue
                    pS, bpS = nps()
                    for cc in range(4):
                        o_ = pS[:, cc * 128:(cc + 1) * 128]
                        em.op("pe", lambda e, cc=cc, o_=o_: e.matmul(o_, lhsT=Bhtok[:, cc * 128:(cc + 1) * 128], rhs=Usb[:, cc * 128:(cc + 1) * 128], start=True, stop=False), r=[BBh, BU_], w=[bpS])
                        em.op("pe", lambda e, cc=cc, o_=o_: e.matmul(o_, lhsT=Khtok[:, cc * 128:(cc + 1) * 128], rhs=Vtok[:, cc * 128:(cc + 1) * 128], start=False, stop=True), r=[BKh, BVt], w=[bpS])
                    for cc in range(4):
                        for hp in range(2):
                            pr_ = slice(hp * 64, hp * 64 + 64)
                            em.op("dve", lambda e, cc=cc, hp=hp, pr_=pr_: e.scalar_tensor_tensor(out=Sst[pr_, cc, :], in0=Sst[pr_, cc, :], scalar=pcs[pr_, cc * 4 + n:cc * 4 + n + 1], in1=pS[pr_, cc * 128 + hp * 64:cc * 128 + hp * 64 + 64], op0=ALU.mult, op1=ALU.add), r=[BS, Bpc, bpS], w=[BS])
                    if rstop <= 6:
                        continue
                    cen, Bcen = tokt["cen"]; sq, Bsq_ = tokt["sq"]; gte, Bgte = tokt["gte"]
                    y3 = Ysb[:, :].rearrange("p (h i) -> p h i", i=64)
                    c3 = cen[:, :].rearrange("p (h i) -> p h i", i=64)
                    s3 = sq[:, :].rearrange("p (h i) -> p h i", i=64)
                    em.op("dve", lambda e: e.tensor_reduce(out=st8[0][:, :], in_=y3, axis=AX.X, op=ALU.add), r=[BY_], w=[Bst8])
                    em.op("dve", lambda e: e.tensor_scalar(out=st8[0][:, :], in0=st8[0][:, :], scalar1=1.0 / 64, scalar2=None, op0=ALU.mult), r=[Bst8], w=[Bst8])
                    em.op("dve", lambda e: e.tensor_tensor(out=c3, in0=y3, in1=st8[0][:, :].unsqueeze(2).to_broadcast([128, 8, 64]), op=ALU.subtract), r=[BY_, Bst8], w=[Bcen])
                    em.op("dve", lambda e: e.tensor_tensor(out=sq[:, :], in0=cen[:, :], in1=cen[:, :], op=ALU.mult), r=[Bcen], w=[Bsq_])
                    em.op("dve", lambda e: e.tensor_reduce(out=st8[1][:, :], in_=s3, axis=AX.X, op=ALU.add), r=[Bsq_], w=[Bst8])
                    em.op("act", lambda e: e.activation(out=st8[1][:, :], in_=st8[1][:, :], func=AF.Sqrt, bias=gneps[:, 0:1], scale=1.0 / 64), r=[Bst8, Bc], w=[Bst8])
                    em.op("dve", lambda e: e.reciprocal(out=st8[1][:, :], in_=st8[1][:, :]), r=[Bst8], w=[Bst8])
                    em.op("dve", lambda e: e.tensor_tensor(out=c3, in0=c3, in1=st8[1][:, :].unsqueeze(2).to_broadcast([128, 8, 64]), op=ALU.mult), r=[Bcen, Bst8], w=[Bcen])
                    em.op("dve", lambda e: e.tensor_tensor(out=cen[:, :], in0=cen[:, :], in1=lnw_bc[:, :], op=ALU.mult), r=[Bcen, Bv], w=[Bcen])
                    em.op("dve", lambda e: e.tensor_tensor(out=cen[:, :], in0=cen[:, :], in1=lnb_bc[:, :], op=ALU.add), r=[Bcen, Bv], w=[Bcen])
                    v3t = Vtok[:, :].rearrange("p (h i) -> p h i", i=64)
                    em.op("dve", lambda e: e.tensor_tensor(out=s3, in0=v3t, in1=bonus[:, n * 8:(n + 1) * 8].unsqueeze(2).to_broadcast([128, 8, 64]), op=ALU.mult), r=[BVt, Bbon], w=[Bsq_])
                    em.op("dve", lambda e: e.tensor_tensor(out=cen[:, :], in0=cen[:, :], in1=sq[:, :], op=ALU.add), r=[Bcen, Bsq_], w=[Bcen])
                    pG, bpG = nps()
                    em.op("pe", lambda e: e.matmul(pG[:, :], lhsT=lorg[:, ts_], rhs=lora_w[:, 1, :], start=True, stop=True), r=[Blo, Bv], w=[bpG])
                    em.op("dve", lambda e: e.tensor_tensor(out=outb[:, :], in0=cen[:, :], in1=pG[:, :], op=ALU.mult), r=[Bcen, bpG], w=[Boutb])
                    pO, bpO = nps()
                    pOb = pO[:, :].bitcast(BF16)
                    for cc in range(4):
                        em.op("pe", lambda e, cc=cc: e.transpose(pOb[:, cc * 128:(cc + 1) * 128], outb[:, cc * 128:(cc + 1) * 128], ident_b[:, :]), r=[Boutb, Bc], w=[bpO])
                    em.op("act", lambda e: e.activation(out=yTb[:, :, :], in_=pOb[:, 0:512].rearrange("p (c t) -> p c t", t=128), func=AF.Copy), r=[bpO], w=[ByTb])
                    tk0 = t0 + n * 128
                    em.dma("sp", ycat_d[4:8, :, tk0:tk0 + 128].rearrange("c p t -> p c t"), yTb[:, :, :], r=[ByTb], w=[B_ycat])
            em.barrier()
        if dbg and dbg[0] == "rwkv":
            break
        with ExitStack() as es:
            knT = sb(es, "knT", [128, 4, S], BF16); kpeT = sb(es, "kpeT", [65, S], BF16); Vsb = sb(es, "Vsb", [128, NT, 512], BF16)
            BK = Buf()
            for h in range(4):
                em.dma("sp", knT[:, h, :], kn_d[h], r=[B_k], w=[BK])
            em.dma("sp", kpeT[0:64, :], kpe_d, r=[B_k], w=[BK])
            em.op("dve", lambda e: e.memset(kpeT[64:65, :], -1.0), w=[BK])
            for j4 in range(4):
                em.dma("sp", Vsb[:, j4 * 8:(j4 + 1) * 8, :], v_d[j4 * 1024:(j4 + 1) * 1024, :].rearrange("(j p) n -> p j n", p=128), r=[B_k], w=[BK])
            gat_col = sb(es, "gat_col", [128, 4])
            em.dma("sp", gat_col[:, :], g_attn_out[l, :].rearrange("(k p) -> p k", p=128), w=[BK], allow_slow_non_contiguous=True)
            kmax2 = sb(es, "kmax2", [128, 4]); kmx = sb(es, "kmx", [128, 1]); Bkm = Buf()
            sq1 = sb(es, "sq1", [128, 512]); sq2 = sb(es, "sq2", [64, 512]); Bsq = Buf()
            em.op("dve", lambda e: e.memset(kmax2[:, :], 0.0), w=[Bkm])
            for g in range(NG):
                em.op("dve", lambda e, g=g: e.tensor_tensor(out=sq2[:, :], in0=kpeT[0:64, g * 512:(g + 1) * 512], in1=kpeT[0:64, g * 512:(g + 1) * 512], op=ALU.mult), r=[BK], w=[Bsq])
                for h in range(4):
                    em.op("dve", lambda e, g=g, h=h: e.tensor_tensor(out=sq1[:, :], in0=knT[:, h, g * 512:(g + 1) * 512], in1=knT[:, h, g * 512:(g + 1) * 512], op=ALU.mult), r=[BK], w=[Bsq])
                    pt, bp = ps[4 + (h % 4)], Bps[4 + (h % 4)]
                    em.op("pe", lambda e, pt=pt: e.matmul(pt[:, :], lhsT=ones_f[:, :], rhs=sq1[:, :], start=True, stop=False), r=[Bc, Bsq], w=[bp])
                    em.op("pe", lambda e, pt=pt: e.matmul(pt[:, :], lhsT=ones_f[0:64, :], rhs=sq2[:, :], start=False, stop=True), r=[Bc, Bsq], w=[bp])
                    em.op("dve", lambda e, pt=pt: e.tensor_reduce(out=kmx[:, :], in_=pt[:, :], axis=AX.X, op=ALU.max), r=[bp], w=[Bkm])
                    em.op("dve", lambda e, h=h: e.tensor_tensor(out=kmax2[:, h:h + 1], in0=kmax2[:, h:h + 1], in1=kmx[:, :], op=ALU.max), r=[Bkm], w=[Bkm])
            qn_s = [sb(es, "qn_s%d" % i, [128, 512], BF16) for i in range(2)]
            qpe_s = [sb(es, "qpe_s%d" % i, [65, 512], BF16) for i in range(2)]
            qsq_s = [sb(es, "qsq_s%d" % i, [65, 512]) for i in range(2)]
            Bqs_ = [Buf(), Buf()]
            PT = [sb(es, "PT%d" % i, [128, 512], BF16) for i in range(4)]; BPT = [Buf() for _ in range(4)]
            dacc = sb(es, "dacc", [128, 512]); Bdacc = Buf()
            oT = [sb(es, "oT%d" % i, [128, 512]) for i in range(4)]; BoT = [Buf() for _ in range(4)]
            rden = sb(es, "rden", [128, 512]); Brd = Buf()
            osq = sb(es, "osq", [128, 512]); Bosq = Buf()
            rs_a = sb(es, "rs_a", [128, 512]); Brs = Buf()
            yst = [sb(es, "yst%d" % i, [128, 512], BF16) for i in range(2)]; Byst = [Buf(), Buf()]
            cnt = 0
            for I in range(NG):
                q0 = I * 512
                for h in range(4):
                    qi = (I * 4 + h) % 2
                    em.dma("sp", qn_s[qi][:, :], qn_d[h, :, q0:q0 + 512], r=[B_q], w=[Bqs_[qi]])
                    em.dma("sp", qpe_s[qi][0:64, :], qpe_d[h, :, q0:q0 + 512], r=[B_q], w=[Bqs_[qi]])
                    em.dma("sp", qsq_s[qi][64:65, :], qsq_d[h:h + 1, q0:q0 + 512], r=[B_q], w=[Bqs_[qi]])
                    em.op("act", lambda e, qi=qi, h=h: e.activation(out=qpe_s[qi][64:65, :], in_=qsq_s[qi][64:65, :], func=AF.Sqrt, scale=kmax2[64:65, h:h + 1]), r=[Bqs_[qi], Bkm], w=[Bqs_[qi]])
                    po, bpo = ps[(h % 2) * 2], Bps[(h % 2) * 2]
                    pd, bpd = ps[(h % 2) * 2 + 1], Bps[(h % 2) * 2 + 1]
                    nj = 4 * I + 4

                    def emit_scores(j):
                        r_ = j - 4 * I
                        c0 = max(r_, 0) * 128
                        k_ = (cnt0 + j)
                        sc, bsc = ps[4 + k_ % 4], Bps[4 + k_ % 4]
                        em.op("pe", lambda e: e.matmul(sc[:, c0:512], lhsT=knT[:, h, j * 128:(j + 1) * 128], rhs=qn_s[qi][:, c0:512], start=True, stop=False), r=[BK, Bqs_[qi]], w=[bsc])
                        em.op("pe", lambda e: e.matmul(sc[:, c0:512], lhsT=kpeT[0:65, j * 128:(j + 1) * 128], rhs=qpe_s[qi][0:65, c0:512], start=False, stop=True), r=[BK, Bqs_[qi]], w=[bsc])
                    cnt0 = cnt
                    emit_scores(0)
                    for j in range(nj):
                        r_ = j - 4 * I
                        c0 = max(r_, 0) * 128
                        pi = cnt % 4
                        sc, bsc = ps[4 + cnt % 4], Bps[4 + cnt % 4]
                        cnt += 1
                        if j + 1 < nj:
                            emit_scores(j + 1)
                        em.op("act", lambda e, sc=sc, c0=c0, pi=pi: e.activation(out=PT[pi][:, c0:512], in_=sc[:, c0:512], func=AF.Exp, scale=SCALE), r=[bsc], w=[BPT[pi]])
                        if r_ >= 0:
                            em.op("dve", lambda e, c0=c0, pi=pi: e.tensor_tensor(out=PT[pi][:, c0:c0 + 128], in0=PT[pi][:, c0:c0 + 128], in1=mincl_b[:, :], op=ALU.mult), r=[BPT[pi], Bc], w=[BPT[pi]])
                        em.op("pe", lambda e, c0=c0, pi=pi, j=j: e.matmul(po[:, c0:512], lhsT=Vsb[:, j, h * 128:(h + 1) * 128], rhs=PT[pi][:, c0:512], start=(j == 0), stop=(j == nj - 1)), r=[BK, BPT[pi]], w=[bpo])
                        if j == 0:
                            em.op("dve", lambda e, pi=pi: e.tensor_copy(dacc[:, :], PT[pi][:, :]), r=[BPT[pi]], w=[Bdacc])
                        else:
                            em.op("dve", lambda e, c0=c0, pi=pi: e.tensor_tensor(out=dacc[:, c0:512], in0=dacc[:, c0:512], in1=PT[pi][:, c0:512], op=ALU.add), r=[BPT[pi], Bdacc], w=[Bdacc])
                    em.op("pe", lambda e: e.matmul(pd[:, :], lhsT=ones_f[:, :], rhs=dacc[:, :], start=True, stop=True), r=[Bc, Bdacc], w=[bpd])
                    em.op("dve", lambda e: e.reciprocal(out=rden[:, :], in_=pd[:, :]), r=[bpd], w=[Brd])
                    em.op("dve", lambda e, h=h: e.tensor_tensor(out=oT[h][:, :], in0=po[:, :], in1=rden[:, :], op=ALU.mult), r=[bpo, Brd], w=[BoT[h]])
                pn, bpn = ps[4], Bps[4]
                for h in range(4):
                    em.op("dve", lambda e, h=h: e.tensor_tensor(out=osq[:, :], in0=oT[h][:, :], in1=oT[h][:, :], op=ALU.mult), r=[BoT[h]], w=[Bosq])
                    em.op("pe", lambda e, h=h: e.matmul(pn[:, :], lhsT=ones_f[:, :], rhs=osq[:, :], start=(h == 0), stop=(h == 3)), r=[Bc, Bosq], w=[bpn])
                rstd_op(rs_a[:, :], pn[:, :], 1.0 / 512, [bpn], [Brs])
                for h in range(4):
                    yi = h % 2
                    em.op("dve", lambda e, h=h, yi=yi: e.scalar_tensor_tensor(out=yst[yi][:, :], in0=oT[h][:, :], scalar=gat_col[:, h:h + 1], in1=rs_a[:, :], op0=ALU.mult, op1=ALU.mult), r=[BoT[h], Brs, BK], w=[Byst[yi]])
                    em.dma("sp", ycat_d[h, :, q0:q0 + 512], yst[yi][:, :], r=[Byst[yi]], w=[B_ycat])
            em.barrier()
        if dbg and dbg[0] == "attn":
            break
        last = (l == depth - 1)
        with ExitStack() as es:
            wout_b = sb(es, "wout_b", [128, 8, D], BF16); Bwo = Buf()
            em.dma("pool", wout_b[:, :, :], w_out[l].rearrange("(k p) n -> p k n", p=128), w=[Bwo])
            wg_b = [sb(es, "wg_b%d" % i, [128, 8, 512], BF16) for i in range(2)]
            wu_b = [sb(es, "wu_b%d" % i, [128, 8, 512], BF16) for i in range(2)]
            wd_b = [sb(es, "wd_b%d" % i, [128, 4, D], BF16) for i in range(2)]
            Bwg = [Buf(), Buf()]; Bwu = [Buf(), Buf()]; Bwd = [Buf(), Buf()]
            TS = 1024
            NTT = TS // 128
            acc = [sb(es, "acc%d" % i, [128, D]) for i in range(NTT)]; Bacc = [Buf() for _ in range(NTT)]
            h2T = sb(es, "h2T", [128, 8, TS], BF16); Bh2 = Buf()
            ycs = [sb(es, "ycs%d" % i, [128, 8, 128], BF16) for i in range(2)]; Bycs = [Buf(), Buf()]
            junk = sb(es, "junk2", [128, D], BF16); ssum = sb(es, "ssum2", [128, 1]); rstd = sb(es, "rstd2", [128, 1])
            xn = sb(es, "xn2", [128, D], BF16); Bnt = Buf()
            actT = [sb(es, "actT%d" % i, [128, 4, TS], BF16) for i in range(2)]; Bact = [Buf(), Buf()]
            sil = [sb(es, "sil%d" % i, [128, 512]) for i in range(2)]; Bsil = [Buf(), Buf()]
            gfin_bc = sb(es, "gfin_bc", [128, D]); Bgf = Buf()
            em.dma("sp", gfin_bc[:, :], g_final[0:1, :].to_broadcast([128, D]), w=[Bgf])
            ost = [sb(es, "ost%d" % i, [128, D]) for i in range(2)]; Bost = [Buf(), Buf()]
            moe = (l % 2 == 1)
            if not moe:
                DFF = 2816
                experts = [(wbf["fg"], wbf["fu"], wbf["fd"], None)]
            else:
                DFF = 3584
                experts = [(wbf["eg"][e_], wbf["eu"][e_], wbf["ed"][e_], e_) for e_ in range(8)]
                h2f = sb(es, "h2f", [128, D]); h2fT = sb(es, "h2fT", [128, 8, 128]); Bh2f = Buf(); Bh2fT = Buf()
                wr_sb = sb(es, "wr_sb", [128, 8, 8]); br_sb = sb(es, "br_sb", [1, 8]); Bwr = Buf()
                em.dma("sp", wr_sb[:, :, :], w_router[0].rearrange("(k p) e -> p k e", p=128), w=[Bwr])
                em.dma("sp", br_sb[:, :], b_router[0:1, :], w=[Bwr])
                lg = [sb(es, "lg%d" % i, [128, 8]) for i in range(5)]; m12 = sb(es, "m12", [128, 4]); Blg = Buf()
                gates = sb(es, "gates", [128, 8 * NTT]); Bgates = Buf()
            groups = [(f0, min(512, DFF - f0)) for f0 in range(0, DFF, 512)]
            gcount = 0
            for g in range(S // TS):
                t0 = g * TS
                for tt in range(NTT):
                    yi = (g * NTT + tt) % 2
                    tk0 = t0 + tt * 128
                    em.dma("sp", ycs[yi][:, :, :], ycat_d[:, :, tk0:tk0 + 128].rearrange("c p t -> p c t"), r=[B_ycat], w=[Bycs[yi]])
                    em.dma("sp", acc[tt][:, :], x_src[tk0:tk0 + 128, :], r=[Bxsrc], w=[Bacc[tt]])
                    for half in range(2):
                        hs_ = slice(half * 512, (half + 1) * 512)
                        pt, bp = nps()
                        for k in range(8):
                            em.op("pe", lambda e, k=k, pt=pt, hs_=hs_: e.matmul(pt[:, :], lhsT=ycs[yi][:, k, :], rhs=wout_b[:, k, hs_], start=(k == 0), stop=(k == 7)), r=[Bycs[yi], Bwo], w=[bp])
                        em.op("dve", lambda e, pt=pt, hs_=hs_: e.tensor_tensor(out=ost[0][:, hs_], in0=pt[:, :], in1=gt1_bc[l][:, hs_], op=ALU.mult), r=[bp, Bmod], w=[Bost[0]])
                        em.op("dve", lambda e, hs_=hs_, tt=tt: e.tensor_tensor(out=acc[tt][:, hs_], in0=ost[0][:, hs_], in1=acc[tt][:, hs_], op=ALU.add), r=[Bost[0], Bacc[tt]], w=[Bacc[tt]])
                    rmsnorm_T(es, acc[tt], Bacc[tt], gs2[l], sh2_col, h2T, Bh2, tt * 128, (junk, ssum, rstd, xn, Bnt))
                    if moe:
                        em.op("dve", lambda e, tt=tt: e.scalar_tensor_tensor(out=h2f[:, :], in0=acc[tt][:, :], scalar=rstd[:, 0:1], in1=gs2_bc[:, :], op0=ALU.mult, op1=ALU.mult), r=[Bacc[tt], Bnt, Bmod], w=[Bh2f])
                        em.op("dve", lambda e: e.tensor_tensor(out=h2f[:, :], in0=h2f[:, :], in1=sh2_bc[:, :], op=ALU.add), r=[Bh2f, Bmod], w=[Bh2f])
                        for hf in range(2):
                            pT_, bpT_ = nps()
                            for k4 in range(4):
                                k = hf * 4 + k4
                                em.op("pe", lambda e, k=k, k4=k4, pT_=pT_: e.transpose(pT_[:, k4 * 128:(k4 + 1) * 128], h2f[:, k * 128:(k + 1) * 128], ident_f[:, :]), r=[Bh2f, Bc], w=[bpT_])
                            em.op("act", lambda e, hf=hf, pT_=pT_: e.activation(out=h2fT[:, hf * 4:(hf + 1) * 4, :], in_=pT_[:, :].rearrange("p (a b) -> p a b", b=128), func=AF.Copy), r=[bpT_], w=[Bh2fT])
                        pl, bpl = nps()
                        for k in range(8):
                            em.op("pe", lambda e, k=k: e.matmul(pl[:, 0:8], lhsT=h2fT[:, k, :], rhs=wr_sb[:, k, :], start=(k == 0), stop=False), r=[Bh2fT, Bwr], w=[bpl])
                        em.op("pe", lambda e: e.matmul(pl[:, 0:8], lhsT=ones_f[0:1, 0:128], rhs=br_sb[0:1, 0:8], start=False, stop=True), r=[Bc, Bwr], w=[bpl])
                        L0, L2, MK1, MK2, TMP = lg
                        em.op("act", lambda e: e.activation(out=L0[:, :], in_=pl[:, 0:8], func=AF.Copy), r=[bpl], w=[Blg])
                        em.op("dve", lambda e: e.tensor_reduce(out=m12[:, 0:1], in_=L0[:, :], axis=AX.X, op=ALU.max), r=[Blg], w=[Blg])
                        em.op("dve", lambda e: e.tensor_scalar(out=MK1[:, :], in0=L0[:, :], scalar1=m12[:, 0:1], scalar2=None, op0=ALU.is_equal), r=[Blg], w=[Blg])
                        em.op("dve", lambda e: e.scalar_tensor_tensor(out=L2[:, :], in0=MK1[:, :], scalar=-1e30, in1=L0[:, :], op0=ALU.mult, op1=ALU.add), r=[Blg], w=[Blg])
                        em.op("dve", lambda e: e.tensor_reduce(out=m12[:, 1:2], in_=L2[:, :], axis=AX.X, op=ALU.max), r=[Blg], w=[Blg])
                        em.op("dve", lambda e: e.tensor_scalar(out=MK2[:, :], in0=L2[:, :], scalar1=m12[:, 1:2], scalar2=None, op0=ALU.is_equal), r=[Blg], w=[Blg])
                        em.op("dve", lambda e: e.tensor_tensor(out=m12[:, 2:3], in0=m12[:, 1:2], in1=m12[:, 0:1], op=ALU.subtract), r=[Blg], w=[Blg])
                        em.op("act", lambda e: e.activation(out=m12[:, 3:4], in_=m12[:, 2:3], func=AF.Sigmoid), r=[Blg], w=[Blg])
                        em.op("act", lambda e: e.activation(out=m12[:, 2:3], in_=m12[:, 2:3], func=AF.Sigmoid, scale=-1.0), r=[Blg], w=[Blg])
                        em.op("dve", lambda e: e.tensor_scalar(out=TMP[:, :], in0=MK1[:, :], scalar1=m12[:, 2:3], scalar2=None, op0=ALU.mult), r=[Blg], w=[Blg])
                        em.op("dve", lambda e, tt=tt: e.scalar_tensor_tensor(out=gates[:, tt * 8:(tt + 1) * 8], in0=MK2[:, :], scalar=m12[:, 3:4], in1=TMP[:, :], op0=ALU.mult, op1=ALU.add), r=[Blg], w=[Bgates])
                for (wG, wU, wD, eidx) in experts:
                    for (f0, fw) in groups:
                        wi = gcount % 2
                        gcount += 1
                        nfc = fw // 128
                        em.dma("sp", wg_b[wi][:, :, 0:fw], wG[:, f0:f0 + fw].rearrange("(k p) n -> p k n", p=128), r=[Bwbf], w=[Bwg[wi]])
                        em.dma("pool", wu_b[wi][:, :, 0:fw], wU[:, f0:f0 + fw].rearrange("(k p) n -> p k n", p=128), r=[Bwbf], w=[Bwu[wi]])
                        em.dma("sp", wd_b[wi][:, 0:nfc, :], wD[f0:f0 + fw, :].rearrange("(c p) n -> p c n", p=128), r=[Bwbf], w=[Bwd[wi]])
                        ai = wi
                        for c in range(nfc):
                            for nh in range(TS // 512):
                                ns_ = slice(nh * 512, (nh + 1) * 512)
                                pg, bpg = nps()
                                pu, bpu = nps()
                                for k in range(8):
                                    em.op("pe", lambda e, k=k, c=c, pg=pg, ns_=ns_: e.matmul(pg[:, :], lhsT=wg_b[wi][:, k, c * 128:(c + 1) * 128], rhs=h2T[:, k, ns_], start=(k == 0), stop=(k == 7)), r=[Bwg[wi], Bh2], w=[bpg])
                                for k in range(8):
                                    em.op("pe", lambda e, k=k, c=c, pu=pu, ns_=ns_: e.matmul(pu[:, :], lhsT=wu_b[wi][:, k, c * 128:(c + 1) * 128], rhs=h2T[:, k, ns_], start=(k == 0), stop=(k == 7)), r=[Bwu[wi], Bh2], w=[bpu])
                                si = (c * 2 + nh) % 2
                                em.op("act", lambda e, pg=pg, si=si: e.activation(out=sil[si][:, :], in_=pg[:, :], func=AF.Silu), r=[bpg], w=[Bsil[si]])
                                em.op("dve", lambda e, pu=pu, si=si, c=c, ns_=ns_: e.tensor_tensor(out=actT[ai][:, c, ns_], in0=sil[si][:, :], in1=pu[:, :], op=ALU.mult), r=[Bsil[si], bpu], w=[Bact[ai]])
                        for tt in range(NTT):
                            for half in range(2):
                                hs_ = slice(half * 512, (half + 1) * 512)
                                pt, bp = nps()
                                for c in range(nfc):
                                    em.op("pe", lambda e, c=c, pt=pt, tt=tt, hs_=hs_: e.matmul(pt[:, :], lhsT=actT[ai][:, c, tt * 128:(tt + 1) * 128], rhs=wd_b[wi][:, c, hs_], start=(c == 0), stop=(c == nfc - 1)), r=[Bact[ai], Bwd[wi]], w=[bp])
                                ti = (tt * 2 + half) % 2
                                if eidx is None:
                                    em.op("dve", lambda e, pt=pt, hs_=hs_, ti=ti: e.tensor_tensor(out=ost[ti][:, 0:512], in0=pt[:, :], in1=gt2_bc[l][:, hs_], op=ALU.mult), r=[bp, Bmod], w=[Bost[ti]])
                                else:
                                    em.op("dve", lambda e, pt=pt, hs_=hs_, ti=ti, tt=tt: e.scalar_tensor_tensor(out=ost[ti][:, 0:512], in0=pt[:, :], scalar=gates[:, tt * 8 + eidx:tt * 8 + eidx + 1], in1=gt2_bc[l][:, hs_], op0=ALU.mult, op1=ALU.mult), r=[bp, Bmod, Bgates], w=[Bost[ti]])
                                em.op("dve", lambda e, hs_=hs_, ti=ti, tt=tt: e.tensor_tensor(out=acc[tt][:, hs_], in0=ost[ti][:, 0:512], in1=acc[tt][:, hs_], op=ALU.add), r=[Bost[ti], Bacc[tt]], w=[Bacc[tt]])
                for tt in range(NTT):
                    tk0 = t0 + tt * 128
                    if not last:
                        em.dma("sp", xres[tk0:tk0 + 128, :], acc[tt][:, :], r=[Bacc[tt]], w=[B_xres])
                    else:
                        oi = tt % 2
                        em.op("act", lambda e, tt=tt: e.activation(out=junk[:, :], in_=acc[tt][:, :], func=AF.Square, accum_out=ssum[:, :]), r=[Bacc[tt]], w=[Bnt])
                        rstd_op(rstd[:, :], ssum[:, :], 1.0 / D, [Bnt], [Bnt])
                        em.op("dve", lambda e, tt=tt, oi=oi: e.scalar_tensor_tensor(out=ost[oi][:, :], in0=acc[tt][:, :], scalar=rstd[:, 0:1], in1=gfin_bc[:, :], op0=ALU.mult, op1=ALU.mult), r=[Bacc[tt], Bnt, Bgf], w=[Bost[oi]])
                        em.dma("sp", out_d[tk0:tk0 + 128, :], ost[oi][:, :], r=[Bost[oi]], w=[B_out])
            em.barrier()

    if dbg:
        em.barrier()
        srcs = {"yrw0": ycat_d[4], "yrw3": ycat_d[7], "ymla": ycat_d[0], "ymla3": ycat_d[3], "qn": qn_d[0], "qpe": qpe_d[0], "kn": kn_d[0], "kpe": kpe_d, "v": v_d, "qsq": qsq_d}
        for nm in dbg[2]:
            src = srcs[nm]
            dd = nc.dram_tensor("dbg_" + nm, list(src.shape), src.dtype, kind="ExternalOutput").ap()
            em.dma("sp", dd, src, w=[B_dbg])
    em.barrier()
    return nc


def _consts():
    i = np.arange(128)
    half = 32
    inv_freq = (10000.0 ** (-(np.arange(half, dtype=np.float32) / np.float32(half)))).astype(np.float32)
    return {
        "c_ident": np.eye(128, dtype=np.float32),
        "c_mincl": (i[:, None] <= i[None, :]).astype(np.float32),
        "c_mstrict": (i[:, None] < i[None, :]).astype(np.float32),
        "c_blk": ((i[:, None] // 64) == (i[None, :] // 64)).astype(np.float32),
        "c_mstrictL": (i[:, None] > i[None, :]).astype(np.float32),
        "c_hsel": ((i[:, None] // 64) == np.arange(2)[None, :]).astype(np.float32),
        "c_invf": np.concatenate([inv_freq, inv_freq]).reshape(64, 1).astype(np.float32),
        "c_sgn": np.concatenate([-np.ones(32), np.ones(32)]).reshape(64, 1).astype(np.float32),
    }


def _in_maps(inputs, depth=DEPTH):
    f = lambda a: np.ascontiguousarray(a)
    shared = {}
    for k in ("w_ada", "b_ada", "g_norm_mix", "g_norm_ffn", "w_in", "w_in_vres", "g_q_norm", "w_uq", "g_kv_norm",
              "w_ukv", "g_attn_out", "mu_shift", "mu_shift_vres", "w0", "w2", "a0", "a2", "g2", "v0", "v2", "k_k",
              "k_a", "ln_x_w", "ln_x_b", "w_out", "w_ffn_gate", "w_ffn_up", "w_ffn_down"):
        shared[k] = f(inputs[k])
    shared["r_k"] = f(np.asarray(inputs["r_k"]).reshape(2, 512))
    shared["g_final"] = f(np.asarray(inputs["g_final"]).reshape(1, D))
    if depth > 1:
        shared["w_router"] = f(inputs["w_router"])
        shared["b_router"] = f(inputs["b_router"])
        shared["w_exp_gate"] = f(np.asarray(inputs["w_exp_gate"])[0])
        shared["w_exp_up"] = f(np.asarray(inputs["w_exp_up"])[0])
        shared["w_exp_down"] = f(np.asarray(inputs["w_exp_down"])[0])
    shared.update(_consts())
    maps = []
    for b in range(8):
        m = dict(shared)
        m["x"] = f(np.asarray(inputs["x"])[b])
        m["c"] = f(np.asarray(inputs["c"])[b:b + 1])
        m["positions"] = f(np.asarray(inputs["positions"])[b:b + 1].astype(np.int32))
        maps.append(m)
    return maps


def kernel(**inputs):
    nc = build()
    maps = _in_maps(inputs)
    res = run_bass_kernel_spmd(nc, maps, core_ids=list(range(8)))
    return np.stack([np.asarray(r["out"]) for r in res.results], axis=0).astype(np.float32)
```

```python
import math
from contextlib import ExitStack

import numpy as np
import concourse.bass as bass
import concourse.mybir as mybir
from concourse.bass_utils import run_bass_kernel_spmd

F32 = mybir.dt.float32
BF16 = mybir.dt.bfloat16
I32 = mybir.dt.int32
AF = mybir.ActivationFunctionType
ALU = mybir.AluOpType
AX = mybir.AxisListType

S = 4096
D = 1024
NT = S // 128
NG = S // 512
DEPTH = 2
MLA_COLS = 448
NDS = 40
NORM_EPS = 1e-6
GN_EPS = 64e-5
SCALE = (128 + 64) ** -0.5
DEC_C = math.exp(-0.5)


class Buf:
    __slots__ = ("w", "r")

    def __init__(self):
        self.w = []
        self.r = []


class Em:
    def __init__(self, nc):
        self.nc = nc
        self.eng = {"pe": nc.tensor, "act": nc.scalar, "dve": nc.vector, "pool": nc.gpsimd, "sp": nc.sync}
        self.sem = {e: nc.semaphore("s_" + e).__enter__() for e in ("pe", "act", "dve", "pool")}
        self.cnt = {e: 0 for e in self.sem}
        self.seen = {e: {} for e in self.eng}
        self.dsems = [nc.semaphore("d%d" % i).__enter__() for i in range(NDS)]
        self.dtot = [0] * NDS
        self.dnext = 0

    def _wait(self, e, tok):
        key, val, sem, src = tok
        if src == "pe" and e == "pe":
            return
        if self.seen[e].get(key, 0) >= val:
            return
        self.eng[e].wait_ge(sem, val)
        self.seen[e][key] = val

    def _deps(self, e, r, w, is_dma=False):
        for b in r:
            for t in b.w:
                self._wait(e, t)
        for b in w:
            for t in b.r:
                self._wait(e, t)
            for t in b.w:
                if is_dma and t[3] == "dma" and not b.r:
                    continue
                self._wait(e, t)

    def _commit(self, tok, r, w, is_dma=False):
        for b in r:
            b.r = [t for t in b.r if t[0] != tok[0]] + [tok]
        for b in w:
            if is_dma and not b.r and b.w and all(t[3] == "dma" for t in b.w):
                b.w = [t for t in b.w if t[0] != tok[0]] + [tok]
            else:
                b.w = [tok]
            b.r = []

    def op(self, e, fn, r=(), w=()):
        self._deps(e, r, w)
        ins = fn(self.eng[e])
        self.cnt[e] += 1
        ins.then_inc(self.sem[e], 1)
        self._commit((e, self.cnt[e], self.sem[e], e), r, w)

    def dma(self, q, out, in_, r=(), w=(), **kw):
        k = self.dnext
        self.dnext = (k + 1) % NDS
        if self.dtot[k]:
            self._wait(q, ("d%d" % k, self.dtot[k], self.dsems[k], "dma"))
        self._deps(q, r, w, is_dma=True)
        ins = self.eng[q].dma_start(out=out, in_=in_, **kw)
        self.dtot[k] += 16
        ins.then_inc(self.dsems[k], 16)
        self._commit(("d%d" % k, self.dtot[k], self.dsems[k], "dma"), r, w, is_dma=True)

    def barrier(self):
        for e in self.eng:
            for f in self.sem:
                if self.cnt[f]:
                    self._wait(e, (f, self.cnt[f], self.sem[f], "bar"))
            for k in range(NDS):
                if self.dtot[k]:
                    self._wait(e, ("d%d" % k, self.dtot[k], self.dsems[k], "dma"))


def build(dbg=None, depth=DEPTH):
    nc = bass.Bass("TRN2", target_bir_lowering=False)
    em = Em(nc)

    def din(name, shape, dt=F32):
        return nc.dram_tensor(name, list(shape), dt, kind="ExternalInput").ap()

    def dscr(name, shape, dt=F32):
        return nc.dram_tensor(name, list(shape), dt).ap()

    x_in = din("x", [S, D])
    c_in = din("c", [1, D])
    pos_in = din("positions", [1, S], I32)
    w_ada = din("w_ada", [2, D, 6 * D])
    b_ada = din("b_ada", [2, 6 * D])
    g_norm_mix = din("g_norm_mix", [2, D])
    g_norm_ffn = din("g_norm_ffn", [2, D])
    w_in = din("w_in", [2, D, 2240])
    w_in_vres = din("w_in_vres", [1, D, 32])
    g_q_norm = din("g_q_norm", [2, 256])
    w_uq = din("w_uq", [2, 256, 768])
    g_kv_norm = din("g_kv_norm", [2, 128])
    w_ukv = din("w_ukv", [2, 128, 1024])
    g_attn_out = din("g_attn_out", [2, 512])
    mu_shift = din("mu_shift", [2, 1792])
    mu_shift_vres = din("mu_shift_vres", [1, 32])
    w0_in = din("w0", [2, 512])
    w2_in = din("w2", [2, 64, 512])
    a0_in = din("a0", [2, 512])
    a2_in = din("a2", [2, 64, 512])
    g2_in = din("g2", [2, 128, 512])
    v0_in = din("v0", [1, 512])
    v2_in = din("v2", [1, 32, 512])
    k_k_in = din("k_k", [2, 512])
    k_a_in = din("k_a", [2, 512])
    r_k_in = din("r_k", [2, 512])
    ln_w_in = din("ln_x_w", [2, 512])
    ln_b_in = din("ln_x_b", [2, 512])
    w_out = din("w_out", [2, D, D])
    w_ffn_gate = din("w_ffn_gate", [1, D, 2816])
    w_ffn_up = din("w_ffn_up", [1, D, 2816])
    w_ffn_down = din("w_ffn_down", [1, 2816, D])
    if depth > 1:
        w_router = din("w_router", [1, D, 8])
        b_router = din("b_router", [1, 8])
        w_exp_gate = din("w_exp_gate", [8, D, 3584])
        w_exp_up = din("w_exp_up", [8, D, 3584])
        w_exp_down = din("w_exp_down", [8, 3584, D])
    g_final = din("g_final", [1, D])
    c_ident = din("c_ident", [128, 128])
    c_mincl = din("c_mincl", [128, 128])
    c_mstrict = din("c_mstrict", [128, 128])
    c_blk = din("c_blk", [128, 128])
    c_mstrictL = din("c_mstrictL", [128, 128])
    c_hsel = din("c_hsel", [128, 2])
    c_invf = din("c_invf", [64, 1])
    c_sgn = din("c_sgn", [64, 1])

    out_d = nc.dram_tensor("out", [S, D], F32, kind="ExternalOutput").ap()
    if dbg:
        dbg_d = nc.dram_tensor("dbg", list(dbg[1]), F32, kind="ExternalOutput").ap()

    xres = dscr("xres", [S, D])
    qn_d = dscr("qn_d", [4, 128, S], BF16)
    qpe_d = dscr("qpe_d", [4, 64, S], BF16)
    qsq_d = dscr("qsq_d", [4, S])
    kn_d = dscr("kn_d", [4, 128, S], BF16)
    kpe_d = dscr("kpe_d", [64, S], BF16)
    v_d = dscr("v_d", [S, 512], BF16)
    ycat_d = dscr("ycat_d", [8, 128, S], BF16)
    vfirst_d = dscr("vfirst_d", [4, 128, S])
    rw_d = dscr("rw_d", [15, 128, S])
    B_rw = Buf()
    B_x = Buf(); B_xres = Buf(); B_q = Buf(); B_k = Buf(); B_ycat = Buf(); B_vf = Buf(); B_out = Buf(); B_dbg = Buf()

    es0 = ExitStack()

    uid = [0]

    def sb(es, name, shape, dt=F32):
        uid[0] += 1
        return es.enter_context(nc.sbuf_tensor("%s_u%d" % (name, uid[0]), list(shape), dt))

    ps = [es0.enter_context(nc.psum_tensor("ps%d" % i, [128, 512], F32)) for i in range(8)]
    Bps = [Buf() for _ in range(8)]
    psrr = [0]

    def nps():
        i = psrr[0]
        psrr[0] = (i + 1) % 8
        return ps[i], Bps[i]

    ident_f = sb(es0, "ident_f", [128, 128]); ident_b = sb(es0, "ident_b", [128, 128], BF16)
    mincl = sb(es0, "mincl", [128, 128]); mstrict = sb(es0, "mstrict", [128, 128]); mincl_b = sb(es0, "mincl_b", [128, 128], BF16)
    mstrictL = sb(es0, "mstrictL", [128, 128]); hsel = sb(es0, "hsel", [128, 2]); gneps = sb(es0, "gneps", [128, 1])
    blk_f = sb(es0, "blk_f", [128, 128]); ones_f = sb(es0, "ones_f", [128, 128]); ones_b = sb(es0, "ones_b", [128, 128], BF16)
    invf = sb(es0, "invf", [64, 1]); sgn = sb(es0, "sgn", [64, 1])
    negpi = sb(es0, "negpi", [128, 1]); epsn = sb(es0, "epsn", [128, 1])
    Bc = Buf()
    for t, s_ in ((ident_f, c_ident), (mincl, c_mincl), (mstrict, c_mstrict), (blk_f, c_blk)):
        em.dma("sp", t[:, :], s_[:, :], w=[Bc])
    em.dma("sp", invf[:, :], c_invf[:, :], w=[Bc])
    em.dma("sp", mstrictL[:, :], c_mstrictL[:, :], w=[Bc])
    em.dma("sp", hsel[:, :], c_hsel[:, :], w=[Bc])
    em.op("dve", lambda e: e.memset(gneps[:, :], GN_EPS), w=[Bc])
    em.dma("sp", sgn[:, :], c_sgn[:, :], w=[Bc])
    em.op("dve", lambda e: e.tensor_copy(ident_b[:, :], ident_f[:, :]), r=[Bc], w=[Bc])
    em.op("dve", lambda e: e.tensor_copy(mincl_b[:, :], mincl[:, :]), r=[Bc], w=[Bc])
    em.op("dve", lambda e: e.memset(ones_f[:, :], 1.0), w=[Bc])
    em.op("dve", lambda e: e.memset(ones_b[:, :], 1.0), w=[Bc])
    em.op("dve", lambda e: e.memset(negpi[:, :], -math.pi), w=[Bc])
    em.op("dve", lambda e: e.memset(epsn[:, :], NORM_EPS), w=[Bc])

    mod_col = [sb(es0, "mod_col%d" % l, [128, 48]) for l in range(depth)]
    gs1 = [sb(es0, "gs1_%d" % l, [128, 8]) for l in range(depth)]
    gs2 = [sb(es0, "gs2_%d" % l, [128, 8]) for l in range(depth)]
    gt1_bc = [sb(es0, "gt1bc%d" % l, [128, D]) for l in range(depth)]
    gt2_bc = [sb(es0, "gt2bc%d" % l, [128, D]) for l in range(depth)]
    gs2_bc = sb(es0, "gs2bc", [128, D]) if depth > 1 else None
    sh2_bc = sb(es0, "sh2bc", [128, D]) if depth > 1 else None
    Bmod = Buf()

    with ExitStack() as es:
        ccol = sb(es, "ccol", [128, 8]); cond = sb(es, "cond", [128, 8])
        cond_bc = sb(es, "cond_bc", [128, 8, 128])
        wada_t = [sb(es, "wada%d" % i, [128, 8, 512]) for i in range(4)]
        Bwada = [Buf() for _ in range(4)]
        mod_bc = sb(es, "mod_bc", [128, 6 * D]); bias_bc = sb(es, "bias_bc", [128, 6 * D])
        tmpd = sb(es, "tmpd", [128, 48, 128])
        gcol = sb(es, "gcol", [128, 8])
        Bl = Buf()
        em.dma("sp", ccol[:, :], c_in[0, :].rearrange("(k p) -> p k", p=128), w=[Bl], allow_slow_non_contiguous=True)
        em.op("act", lambda e: e.activation(out=cond[:, :], in_=ccol[:, :], func=AF.Silu), r=[Bl], w=[Bl])
        em.op("dve", lambda e: e.tensor_copy(cond_bc[:, :, :], cond[:, :].unsqueeze(2).to_broadcast([128, 8, 128])), r=[Bl], w=[Bl])
        for l in range(depth):
            em.dma("sp", bias_bc[:, :], b_ada[l:l + 1, :].partition_broadcast(128) if False else b_ada[l:l + 1, :].to_broadcast([128, 6 * D]), w=[Bl])
            for gI in range(12):
                wt, bw = wada_t[gI % 4], Bwada[gI % 4]
                em.dma("sp", wt[:, 0:4, :], w_ada[l, 0:512, gI * 512:(gI + 1) * 512].rearrange("(k p) n -> p k n", p=128), w=[bw])
                em.dma("sp", wt[:, 4:8, :], w_ada[l, 512:1024, gI * 512:(gI + 1) * 512].rearrange("(k p) n -> p k n", p=128), w=[bw])
                pt, bp = nps()
                for k in range(8):
                    em.op("pe", lambda e, k=k: e.matmul(pt[:, :], lhsT=cond_bc[:, k, :], rhs=wt[:, k, :], start=(k == 0), stop=(k == 7)), r=[Bl, bw], w=[bp])
                em.op("dve", lambda e: e.tensor_tensor(out=mod_bc[:, gI * 512:(gI + 1) * 512], in0=pt[:, :], in1=bias_bc[:, gI * 512:(gI + 1) * 512], op=ALU.add), r=[bp, Bl], w=[Bl])
            em.op("dve", lambda e: e.tensor_tensor(out=tmpd[:, :, :], in0=mod_bc[:, :].rearrange("p (a b) -> p a b", b=128), in1=ident_f[:, :].unsqueeze(1).to_broadcast([128, 48, 128]), op=ALU.mult), r=[Bl, Bc], w=[Bl])
            em.op("dve", lambda e: e.tensor_reduce(out=mod_col[l][:, :], in_=tmpd[:, :, :], axis=AX.X, op=ALU.add), r=[Bl], w=[Bmod])
            for (gsrc, scv, gdst) in ((g_norm_mix, 1, gs1[l]), (g_norm_ffn, 4, gs2[l])):
                em.dma("sp", gcol[:, :], gsrc[l, :].rearrange("(k p) -> p k", p=128), w=[Bl], allow_slow_non_contiguous=True)
                em.op("dve", lambda e: e.scalar_tensor_tensor(out=gdst[:, :], in0=mod_col[l][:, scv * 8:scv * 8 + 8], scalar=1.0, in1=gcol[:, :], op0=ALU.add, op1=ALU.mult), r=[Bl, Bmod], w=[Bmod])
            em.op("dve", lambda e: e.tensor_copy(gt1_bc[l][:, :], mod_bc[:, 2 * D:3 * D]), r=[Bl], w=[Bmod])
            em.op("dve", lambda e: e.tensor_copy(gt2_bc[l][:, :], mod_bc[:, 5 * D:6 * D]), r=[Bl], w=[Bmod])
            if l == 1:
                gbc = sb(es, "gbc", [128, D])
                em.dma("sp", gbc[:, :], g_norm_ffn[l:l + 1, :].to_broadcast([128, D]), w=[Bl])
                em.op("dve", lambda e: e.scalar_tensor_tensor(out=gs2_bc[:, :], in0=mod_bc[:, 4 * D:5 * D], scalar=1.0, in1=gbc[:, :], op0=ALU.add, op1=ALU.mult), r=[Bl], w=[Bmod])
                em.op("dve", lambda e: e.tensor_copy(sh2_bc[:, :], mod_bc[:, 3 * D:4 * D]), r=[Bl], w=[Bmod])
        em.barrier()

    def rstd_op(dst, src, scale, rd, wr, rows=128):
        em.op("act", lambda e: e.activation(out=dst, in_=src, func=AF.Sqrt, bias=epsn[0:rows, 0:1], scale=scale), r=list(rd) + [Bc], w=list(wr))
        em.op("dve", lambda e: e.reciprocal(out=dst, in_=dst), r=list(wr), w=list(wr))

    def rmsnorm_T(es_pool, xt, Bxt, gs_col, sh_col, hT_dst, Bh, col0, tagbuf):
        junk, ssum, rstd, xn, Bt = tagbuf
        em.op("act", lambda e: e.activation(out=junk[:, :], in_=xt[:, :], func=AF.Square, accum_out=ssum[:, :]), r=[Bxt], w=[Bt])
        rstd_op(rstd[:, :], ssum[:, :], 1.0 / D, [Bt], [Bt])
        em.op("act", lambda e: e.activation(out=xn[:, :], in_=xt[:, :], func=AF.Copy, scale=rstd[:, 0:1]), r=[Bxt, Bt], w=[Bt])
        pt, bp = nps()
        ptb = pt[:, :].bitcast(BF16)
        for k in range(8):
            em.op("pe", lambda e, k=k: e.transpose(ptb[:, k * 128:(k + 1) * 128], xn[:, k * 128:(k + 1) * 128], ident_b[:, :]), r=[Bt, Bc], w=[bp])
        for k in range(8):
            eng = "dve" if k % 2 == 0 else "act"
            if eng == "dve":
                em.op("dve", lambda e, k=k: e.tensor_scalar(out=hT_dst[:, k, col0:col0 + 128], in0=ptb[:, k * 128:(k + 1) * 128], scalar1=gs_col[:, k:k + 1], scalar2=sh_col[:, k:k + 1], op0=ALU.mult, op1=ALU.add), r=[bp, Bmod], w=[Bh])
            else:
                em.op("act", lambda e, k=k: e.activation(out=hT_dst[:, k, col0:col0 + 128], in_=ptb[:, k * 128:(k + 1) * 128], func=AF.Identity, scale=gs_col[:, k:k + 1], bias=sh_col[:, k:k + 1]), r=[bp, Bmod], w=[Bh])

    Bwbf = Buf()
    wbf = {}

    def conv_w(name, src2d):
        R_, C_ = src2d.shape
        dst = dscr(name, [R_, C_], BF16)
        for r0 in range(0, R_, 256):
            r1 = min(R_, r0 + 256)
            em.dma("pool", dst[r0:r1, :], src2d[r0:r1, :], w=[Bwbf])
        return dst
    wbf["fg"] = conv_w("wbf_fg", w_ffn_gate[0]); wbf["fu"] = conv_w("wbf_fu", w_ffn_up[0]); wbf["fd"] = conv_w("wbf_fd", w_ffn_down[0])
    if depth > 1:
        wbf["eg"] = [conv_w("wbf_eg%d" % e_, w_exp_gate[e_]) for e_ in range(8)]
        wbf["eu"] = [conv_w("wbf_eu%d" % e_, w_exp_up[e_]) for e_ in range(8)]
        wbf["ed"] = [conv_w("wbf_ed%d" % e_, w_exp_down[e_]) for e_ in range(8)]

    for l in range(depth):
        x_src = x_in if l == 0 else xres
        Bxsrc = B_x if l == 0 else B_xres
        sh1_col = mod_col[l][:, 0:8]
        sh2_col = mod_col[l][:, 24:32]
        NCH = 18 if l == 0 else 19
        with ExitStack() as es:
            wcat = sb(es, "wcat", [128, 8, 19 * 128], BF16); Bw = Buf()
            Bwst = Buf()

            def load_cols(dst_c0, src_ap_fn, ncols):
                em.dma("pool", wcat[:, :, dst_c0:dst_c0 + ncols], src_ap_fn, w=[Bw])

            wv = w_in[l].rearrange("(k p) n -> p k n", p=128)
            load_cols(0, wv[:, :, 0:448], 448)
            load_cols(448, wv[:, :, 416:448], 32)
            load_cols(480, wv[:, :, 384:416], 32)
            for j in range(3):
                load_cols(512 + j * 512, wv[:, :, 448 + j * 512:448 + (j + 1) * 512], 512)
            load_cols(2048, wv[:, :, 1984:2240], 256)
            if l == 1:
                load_cols(2304, w_in_vres[0].rearrange("(k p) n -> p k n", p=128), 32)
            wuq = sb(es, "wuq", [128, 2, 4, 256], BF16)
            wq = w_uq[l].rearrange("(k p) n -> p k n", p=128)
            for h in range(4):
                em.dma("pool", wuq[:, :, h, 0:192], wq[:, :, h * 192:(h + 1) * 192], w=[Bw])
                em.dma("pool", wuq[:, :, h, 192:224], wq[:, :, h * 192 + 160:h * 192 + 192], w=[Bw])
                em.dma("pool", wuq[:, :, h, 224:256], wq[:, :, h * 192 + 128:h * 192 + 160], w=[Bw])
            wukv = sb(es, "wukv", [128, 1024], BF16)
            for h in range(4):
                em.dma("pool", wukv[:, h * 128:(h + 1) * 128], w_ukv[l][:, h * 256:h * 256 + 128], w=[Bw])
                em.dma("pool", wukv[:, 512 + h * 128:512 + (h + 1) * 128], w_ukv[l][:, h * 256 + 128:h * 256 + 256], w=[Bw])
            def colvec(name, src_row, n):
                t = sb(es, name, [128, n])
                em.dma("sp", t[:, :], src_row.rearrange("(k p) -> p k", p=128), w=[Bw], allow_slow_non_contiguous=True)
                return t
            gq_col = colvec("gq_col", g_q_norm[l, :], 2)
            gkv_col = colvec("gkv_col", g_kv_norm[l, :], 1)
            mu_col = colvec("mu_col", mu_shift[l, :], 14)
            if l == 1:
                muv_col = sb(es, "muv_col", [32, 1]); omuv_col = sb(es, "omuv_col", [32, 1])
                em.dma("sp", muv_col[:, :], mu_shift_vres[0, :].rearrange("(p o) -> p o", o=1), w=[Bw])
                em.op("dve", lambda e: e.tensor_scalar(out=omuv_col[:, :], in0=muv_col[:, :], scalar1=-1.0, scalar2=1.0, op0=ALU.mult, op1=ALU.add), r=[Bw], w=[Bw])
            omu_col = sb(es, "omu_col", [128, 14])
            em.op("dve", lambda e: e.tensor_scalar(out=omu_col[:, :], in0=mu_col[:, :], scalar1=-1.0, scalar2=1.0, op0=ALU.mult, op1=ALU.add), r=[Bw], w=[Bw])

            xt = [sb(es, "xt%d" % i, [128, D]) for i in range(2)]; Bxt = [Buf(), Buf()]
            junk = sb(es, "junk", [128, D], BF16); ssum = sb(es, "ssum", [128, 1]); rstd = sb(es, "rstd", [128, 1])
            xn = sb(es, "xn", [128, D], BF16); Bnt = Buf()
            hT = sb(es, "hT", [128, 8, 512], BF16); BhT = Buf()
            pos_i = sb(es, "pos_i", [64, 512], I32); pos_f = sb(es, "pos_f", [64, 512])
            ang = sb(es, "ang", [64, 512]); cos2 = sb(es, "cos2", [64, 512]); sin2 = sb(es, "sin2", [64, 512]); Brope = Buf()
            mla_t = [sb(es, "mla_t%d" % i, [128, 512]) for i in range(6)]; Bm = [Buf() for _ in range(6)]
            cqn = sb(es, "cqn", [128, 2, 512], BF16); ckvn = sb(es, "ckvn", [128, 512], BF16); Bcn = Buf()
            stg_b = [sb(es, "stg_b%d" % i, [128, 512], BF16) for i in range(4)]; Bstg = [Buf() for _ in range(4)]
            stg_i = [0]
            qsq_row = sb(es, "qsq_row", [1, 512]); Bqs = Buf()
            praw = [sb(es, "praw%d" % i, [128, 513]) for i in range(2)]; Bpraw = [Buf(), Buf()]
            carry = sb(es, "carry", [128, 19]); Bcar = Buf()
            em.op("dve", lambda e: e.memset(carry[:, :], 0.0), w=[Bcar])
            rwst = [sb(es, "rwst%d" % i, [128, 512]) for i in range(2)]; Brwst = [Buf(), Buf()]
            rwtmp = sb(es, "rwtmp", [128, 512]); Brwtmp = Buf()

            def stg():
                i = stg_i[0]
                stg_i[0] = (i + 1) % 4
                return stg_b[i], Bstg[i]

            for g in range(NG):
                t0 = g * 512
                for tt in range(4):
                    xi = (g * 4 + tt) % 2
                    em.dma("sp", xt[xi][:, :], x_src[t0 + tt * 128:t0 + (tt + 1) * 128, :], r=[Bxsrc], w=[Bxt[xi]])
                    rmsnorm_T(es, xt[xi], Bxt[xi], gs1[l], sh1_col, hT, BhT, tt * 128, (junk, ssum, rstd, xn, Bnt))
                em.dma("sp", pos_i[:, :], pos_in[0:1, t0:t0 + 512].to_broadcast([64, 512]), w=[Brope])
                em.op("dve", lambda e: e.tensor_copy(pos_f[:, :], pos_i[:, :]), r=[Brope], w=[Brope])
                em.op("dve", lambda e: e.tensor_scalar(out=ang[:, :], in0=pos_f[:, :], scalar1=invf[:, 0:1], scalar2=None, op0=ALU.mult), r=[Brope, Bc], w=[Brope])
                def sintab(dst, off):
                    em.op("dve", lambda e: e.tensor_scalar(out=pos_f[:, :], in0=ang[:, :], scalar1=1.0 / (2 * math.pi), scalar2=off, op0=ALU.mult, op1=ALU.add), r=[Brope], w=[Brope])
                    em.op("dve", lambda e: e.tensor_copy(pos_i[:, :], pos_f[:, :]), r=[Brope], w=[Brope])
                    em.op("dve", lambda e: e.tensor_copy(dst[:, :], pos_i[:, :]), r=[Brope], w=[Brope])
                    em.op("dve", lambda e: e.tensor_tensor(out=pos_f[:, :], in0=pos_f[:, :], in1=dst[:, :], op=ALU.subtract), r=[Brope], w=[Brope])
                    em.op("dve", lambda e: e.tensor_scalar(out=dst[:, :], in0=pos_f[:, :], scalar1=0.5, scalar2=None, op0=ALU.is_gt), r=[Brope], w=[Brope])
                    em.op("dve", lambda e: e.tensor_tensor(out=pos_f[:, :], in0=pos_f[:, :], in1=dst[:, :], op=ALU.subtract), r=[Brope], w=[Brope])
                    em.op("dve", lambda e: e.tensor_scalar(out=dst[:, :], in0=pos_f[:, :], scalar1=-0.5, scalar2=None, op0=ALU.is_lt), r=[Brope], w=[Brope])
                    em.op("dve", lambda e: e.tensor_tensor(out=pos_f[:, :], in0=pos_f[:, :], in1=dst[:, :], op=ALU.add), r=[Brope], w=[Brope])
                    em.op("act", lambda e: e.activation(out=dst[:, :], in_=pos_f[:, :], func=AF.Sin, scale=2 * math.pi), r=[Brope], w=[Brope])
                sintab(cos2, 0.25)
                sintab(sin2, 0.0)
                em.op("dve", lambda e: e.tensor_scalar(out=sin2[:, :], in0=sin2[:, :], scalar1=sgn[:, 0:1], scalar2=None, op0=ALU.mult), r=[Brope, Bc], w=[Brope])

                def proj_chunk(c0, m, use_rows=128):
                    pt, bp = nps()
                    for k in range(8):
                        em.op("pe", lambda e, k=k: e.matmul(pt[0:m, :], lhsT=wcat[:, k, c0:c0 + m], rhs=hT[:, k, :], start=(k == 0), stop=(k == 7)), r=[Bw, BhT], w=[bp])
                    return pt, bp

                cq_f = [mla_t[0], mla_t[1]]
                for j in range(2):
                    pt, bp = proj_chunk(j * 128, 128)
                    em.op("act", lambda e, j=j, pt=pt: e.activation(out=cq_f[j][:, :], in_=pt[:, :], func=AF.Copy), r=[bp], w=[Bm[j]])
                    em.op("dve", lambda e, j=j: e.tensor_tensor(out=mla_t[2 + j][:, :], in0=cq_f[j][:, :], in1=cq_f[j][:, :], op=ALU.mult), r=[Bm[j]], w=[Bm[2 + j]])
                pss, bpss = nps()
                for j in range(2):
                    em.op("pe", lambda e, j=j: e.matmul(pss[:, :], lhsT=ones_f[:, :], rhs=mla_t[2 + j][:, :], start=(j == 0), stop=(j == 1)), r=[Bc, Bm[2 + j]], w=[bpss])
                rstd_op(mla_t[4][:, :], pss[:, :], 1.0 / 256, [bpss], [Bm[4]])
                for j in range(2):
                    em.op("dve", lambda e, j=j: e.scalar_tensor_tensor(out=cqn[:, j, :], in0=cq_f[j][:, :], scalar=gq_col[:, j:j + 1], in1=mla_t[4][:, :], op0=ALU.mult, op1=ALU.mult), r=[Bm[j], Bm[4], Bw], w=[Bcn])
                pt, bp = proj_chunk(256, 128)
                em.op("act", lambda e, pt=pt: e.activation(out=mla_t[0][:, :], in_=pt[:, :], func=AF.Copy), r=[bp], w=[Bm[0]])
                em.op("dve", lambda e: e.tensor_tensor(out=mla_t[2][:, :], in0=mla_t[0][:, :], in1=mla_t[0][:, :], op=ALU.mult), r=[Bm[0]], w=[Bm[2]])
                pss, bpss = nps()
                em.op("pe", lambda e: e.matmul(pss[:, :], lhsT=ones_f[:, :], rhs=mla_t[2][:, :], start=True, stop=True), r=[Bc, Bm[2]], w=[bpss])
                rstd_op(mla_t[4][:, :], pss[:, :], 1.0 / 128, [bpss], [Bm[4]])
                em.op("dve", lambda e: e.scalar_tensor_tensor(out=ckvn[:, :], in0=mla_t[0][:, :], scalar=gkv_col[:, 0:1], in1=mla_t[4][:, :], op0=ALU.mult, op1=ALU.mult), r=[Bm[0], Bm[4], Bw], w=[Bcn])
                pk, bpk = proj_chunk(384, 64)
                pks, bpks = proj_chunk(448, 64)
                em.op("dve", lambda e: e.tensor_tensor(out=mla_t[0][0:64, :], in0=pk[0:64, :], in1=cos2[:, :], op=ALU.mult), r=[bpk, Brope], w=[Bm[0]])
                em.op("dve", lambda e: e.tensor_tensor(out=mla_t[1][0:64, :], in0=pks[0:64, :], in1=sin2[:, :], op=ALU.mult), r=[bpks, Brope], w=[Bm[1]])
                sg, bsg = stg()
                em.op("dve", lambda e: e.tensor_tensor(out=sg[0:64, :], in0=mla_t[0][0:64, :], in1=mla_t[1][0:64, :], op=ALU.add), r=[Bm[0], Bm[1]], w=[bsg])
                em.dma("sp", kpe_d[:, t0:t0 + 512], sg[0:64, :], r=[bsg], w=[B_k])
                for h in range(4):
                    pt, bp = nps()
                    em.op("pe", lambda e, h=h, pt=pt: e.matmul(pt[:, :], lhsT=wukv[:, h * 128:(h + 1) * 128], rhs=ckvn[:, :], start=True, stop=True), r=[Bw, Bcn], w=[bp])
                    sg, bsg = stg()
                    em.op("act", lambda e, pt=pt, sg=sg: e.activation(out=sg[:, :], in_=pt[:, :], func=AF.Copy), r=[bp], w=[bsg])
                    em.dma("sp", kn_d[h, :, t0:t0 + 512], sg[:, :], r=[bsg], w=[B_k])
                for tt in range(4):
                    pt, bp = nps()
                    em.op("pe", lambda e, tt=tt, pt=pt: e.matmul(pt[:, :], lhsT=ckvn[:, tt * 128:(tt + 1) * 128], rhs=wukv[:, 512:1024], start=True, stop=True), r=[Bw, Bcn], w=[bp])
                    sg, bsg = stg()
                    em.op("act", lambda e, pt=pt, sg=sg: e.activation(out=sg[:, :], in_=pt[:, :], func=AF.Copy), r=[bp], w=[bsg])
                    em.dma("sp", v_d[t0 + tt * 128:t0 + (tt + 1) * 128, :], sg[:, :], r=[bsg], w=[B_k])
                for h in range(4):
                    pq, bpq = nps()
                    for k in range(2):
                        em.op("pe", lambda e, k=k, h=h, pq=pq: e.matmul(pq[:, :], lhsT=wuq[:, k, h, 0:128], rhs=cqn[:, k, :], start=(k == 0), stop=(k == 1)), r=[Bw, Bcn], w=[bpq])
                    sg, bsg = stg()
                    em.op("act", lambda e, pq=pq, sg=sg: e.activation(out=sg[:, :], in_=pq[:, :], func=AF.Copy), r=[bpq], w=[bsg])
                    em.dma("sp", qn_d[h, :, t0:t0 + 512], sg[:, :], r=[bsg], w=[B_q])
                    em.op("dve", lambda e, pq=pq: e.tensor_tensor(out=mla_t[2][:, :], in0=sg[:, :], in1=sg[:, :], op=ALU.mult), r=[bsg], w=[Bm[2]])
                    pp, bpp = nps()
                    pp2, bpp2 = nps()
                    for k in range(2):
                        em.op("pe", lambda e, k=k, h=h, pp=pp: e.matmul(pp[0:64, :], lhsT=wuq[:, k, h, 128:192], rhs=cqn[:, k, :], start=(k == 0), stop=(k == 1)), r=[Bw, Bcn], w=[bpp])
                    for k in range(2):
                        em.op("pe", lambda e, k=k, h=h, pp2=pp2: e.matmul(pp2[0:64, :], lhsT=wuq[:, k, h, 192:256], rhs=cqn[:, k, :], start=(k == 0), stop=(k == 1)), r=[Bw, Bcn], w=[bpp2])
                    em.op("dve", lambda e, pp=pp: e.tensor_tensor(out=mla_t[0][0:64, :], in0=pp[0:64, :], in1=cos2[:, :], op=ALU.mult), r=[bpp, Brope], w=[Bm[0]])
                    em.op("dve", lambda e, pp2=pp2: e.tensor_tensor(out=mla_t[1][0:64, :], in0=pp2[0:64, :], in1=sin2[:, :], op=ALU.mult), r=[bpp2, Brope], w=[Bm[1]])
                    sg2, bsg2 = stg()
                    em.op("dve", lambda e, sg2=sg2: e.tensor_tensor(out=sg2[0:64, :], in0=mla_t[0][0:64, :], in1=mla_t[1][0:64, :], op=ALU.add), r=[Bm[0], Bm[1]], w=[bsg2])
                    em.dma("sp", qpe_d[h, :, t0:t0 + 512], sg2[0:64, :], r=[bsg2], w=[B_q])
                    em.op("dve", lambda e, sg2=sg2: e.tensor_tensor(out=mla_t[3][0:64, :], in0=sg2[0:64, :], in1=sg2[0:64, :], op=ALU.mult), r=[bsg2], w=[Bm[3]])
                    pr, bpr = nps()
                    em.op("pe", lambda e, pr=pr: e.matmul(pr[0:1, :], lhsT=ones_f[:, 0:1], rhs=mla_t[2][:, :], start=True, stop=False), r=[Bc, Bm[2]], w=[bpr])
                    em.op("pe", lambda e, pr=pr: e.matmul(pr[0:1, :], lhsT=ones_f[0:64, 0:1], rhs=mla_t[3][0:64, :], start=False, stop=True), r=[Bc, Bm[3]], w=[bpr])
                    em.op("act", lambda e, pr=pr: e.activation(out=qsq_row[:, :], in_=pr[0:1, :], func=AF.Copy), r=[bpr], w=[Bqs])
                    em.dma("sp", qsq_d[h:h + 1, t0:t0 + 512], qsq_row[:, :], r=[Bqs], w=[B_q])

                if dbg and dbg[0] == "mla_prep":
                    continue
                nrw = 14 if l == 0 else 15
                for ci in range(nrw):
                    rows = 128 if ci < 14 else 32
                    c0 = 512 + ci * 128 if ci < 14 else 2304
                    pt, bp = proj_chunk(c0, rows)
                    pi_ = ci % 2
                    pr_, bpr_ = praw[pi_], Bpraw[pi_]
                    em.op("act", lambda e, pt=pt, pr_=pr_, rows=rows: e.activation(out=pr_[0:rows, 1:513], in_=pt[0:rows, :], func=AF.Copy), r=[bp], w=[bpr_])
                    em.op("dve", lambda e, pr_=pr_, rows=rows, ci=ci: e.tensor_copy(pr_[0:rows, 0:1], carry[0:rows, ci:ci + 1]), r=[Bcar], w=[bpr_])
                    mu_ap = mu_col[:, ci:ci + 1] if ci < 14 else muv_col[:, 0:1]
                    omu_ap = omu_col[:, ci:ci + 1] if ci < 14 else omuv_col[:, 0:1]
                    em.op("dve", lambda e, pr_=pr_, rows=rows, omu_ap=omu_ap: e.tensor_scalar(out=rwtmp[0:rows, :], in0=pr_[0:rows, 1:513], scalar1=omu_ap, scalar2=None, op0=ALU.mult), r=[bpr_, Bw], w=[Brwtmp])
                    ri = ci % 2
                    em.op("dve", lambda e, pr_=pr_, rows=rows, mu_ap=mu_ap, ri=ri: e.scalar_tensor_tensor(out=rwst[ri][0:rows, :], in0=pr_[0:rows, 0:512], scalar=mu_ap, in1=rwtmp[0:rows, :], op0=ALU.mult, op1=ALU.add), r=[bpr_, Brwtmp, Bw], w=[Brwst[ri]])
                    em.op("dve", lambda e, pr_=pr_, rows=rows, ci=ci: e.tensor_copy(carry[0:rows, ci:ci + 1], pr_[0:rows, 512:513]), r=[bpr_], w=[Bcar])
                    em.dma("sp", rw_d[ci, 0:rows, t0:t0 + 512], rwst[ri][0:rows, :], r=[Brwst[ri]], w=[B_rw])
            em.barrier()
        if dbg and dbg[0] == "mla_prep":
            break
        with ExitStack() as es:
            Bv = Buf()

            def colvec2(name, src_row, n):
                t = sb(es, name, [128, n])
                em.dma("sp", t[:, :], src_row.rearrange("(k p) -> p k", p=128), w=[Bv], allow_slow_non_contiguous=True)
                return t
            w0_col = colvec2("w0_col", w0_in[l, :], 4)
            a0_col = colvec2("a0_col", a0_in[l, :], 4)
            kk_col = colvec2("kk_col", k_k_in[l, :], 4)
            ka_col = colvec2("ka_col", k_a_in[l, :], 4)
            rk_col = colvec2("rk_col", r_k_in[l, :], 4)
            lora_w = sb(es, "lora_w", [128, 2, 512])
            lora_a2 = sb(es, "lora_a2", [64, 512]); lora_ain = sb(es, "lora_ain", [64, 512])
            em.dma("sp", lora_w[0:64, 0, :], w2_in[l], w=[Bv])
            em.dma("sp", lora_a2[:, :], a2_in[l], w=[Bv])
            em.dma("sp", lora_w[:, 1, :], g2_in[l], w=[Bv])
            lnw_bc = sb(es, "lnw_bc", [128, 512]); lnb_bc = sb(es, "lnb_bc", [128, 512])
            em.dma("sp", lnw_bc[:, :], ln_w_in[l:l + 1, :].to_broadcast([128, 512]), w=[Bv])
            em.dma("sp", lnb_bc[:, :], ln_b_in[l:l + 1, :].to_broadcast([128, 512]), w=[Bv])
            if l == 1:
                v0_col = colvec2("v0_col", v0_in[0, :], 4)
                v2_sb = sb(es, "v2_sb", [32, 512])
                em.dma("sp", v2_sb[:, :], v2_in[0], w=[Bv])
                vl = sb(es, "vl", [32, 512])
            big = {}
            for nm in ("R", "K", "V", "LW", "CA", "CB", "AS", "T1", "T2", "T3", "T4", "T5"):
                big[nm] = (sb(es, "big_" + nm, [128, 4, 512]), Buf())
            lorwa = sb(es, "lorwa", [128, 512]); lorg = sb(es, "lorg", [128, 512]); tw = sb(es, "tw", [64, 512]); Blo = Buf()
            Sst = sb(es, "Sst", [128, 4, 64]); BS = Buf()
            em.op("dve", lambda e: e.memset(Sst[:, :, :], 0.0), w=[BS])
            pcs = sb(es, "pcs", [128, 16]); Bpc = Buf()
            mats = {}
            for nm in ("N", "M", "AkT", "GbT", "GkT", "Tt", "X2", "XT2"):
                mats[nm] = (sb(es, "mat_" + nm, [128, 8, 128], F32), [Buf(), Buf()])
            tokt = {}
            for nm in ("Vtok", "Bhtok", "Khtok", "Wsb", "Usb", "Ysb", "cen", "sq", "gte"):
                tokt[nm] = (sb(es, "tok_" + nm, [128, 512], F32), Buf())
            bonus = sb(es, "bonus", [128, 32]); Bbon = Buf()
            st8 = [sb(es, "st8_%d" % i, [128, 8]) for i in range(3)]; Bst8 = Buf()
            outb = sb(es, "outb", [128, 512], BF16); Boutb = Buf()
            yTb = sb(es, "yTb", [128, 4, 128], BF16); ByTb = Buf()

            def v3(t):
                return t[:, :, :].rearrange("p c (n t) -> p (c n) t", t=128)

            def fl(t):
                return t[:, :, :].rearrange("p c t -> p (c t)")
            (R_, BR), (K_, BK_), (V_, BV_), (LW, BLW), (CA, BCA), (CB, BCB) = (big[n] for n in ("R", "K", "V", "LW", "CA", "CB"))
            (AS, BAS), (T1, BT1), (T2, BT2), (T3, BT3), (T4, BT4), (T5, BT5) = (big[n] for n in ("AS", "T1", "T2", "T3", "T4", "T5"))

            rstop = dbg[3] if (dbg and len(dbg) > 3) else 99
            for g in range(NG):
                if rstop < 99 and g > 0:
                    continue
                t0 = g * 512
                for cc in range(4):
                    em.dma("sp", R_[:, cc, :], rw_d[cc, :, t0:t0 + 512], r=[B_rw], w=[BR])
                    em.dma("sp", K_[:, cc, :], rw_d[4 + cc, :, t0:t0 + 512], r=[B_rw], w=[BK_])
                    em.dma("sp", V_[:, cc, :], rw_d[8 + cc, :, t0:t0 + 512], r=[B_rw], w=[BV_])
                em.dma("sp", lorwa[:, :], rw_d[12, :, t0:t0 + 512], r=[B_rw], w=[Blo])
                em.dma("sp", lorg[:, :], rw_d[13, :, t0:t0 + 512], r=[B_rw], w=[Blo])
                em.dma("sp", lora_ain[:, :], rw_d[12, 64:128, t0:t0 + 512], r=[B_rw], w=[Blo])
                em.op("act", lambda e: e.activation(out=tw[:, :], in_=lorwa[0:64, :], func=AF.Tanh), r=[Blo], w=[Blo])
                for cc in range(4):
                    pw, bpw = nps()
                    em.op("pe", lambda e, cc=cc, pw=pw: e.matmul(pw[:, :], lhsT=lora_w[0:64, 0, cc * 128:(cc + 1) * 128], rhs=tw[0:64, :], start=True, stop=True), r=[Bv, Blo], w=[bpw])
                    em.op("act", lambda e, cc=cc, pw=pw: e.activation(out=LW[:, cc, :], in_=pw[:, :], func=AF.Sigmoid, bias=w0_col[:, cc:cc + 1]), r=[bpw, Bv], w=[BLW])
                    pa, bpa = nps()
                    em.op("pe", lambda e, cc=cc, pa=pa: e.matmul(pa[:, :], lhsT=lora_a2[0:64, cc * 128:(cc + 1) * 128], rhs=lora_ain[0:64, :], start=True, stop=True), r=[Bv, Blo], w=[bpa])
                    em.op("act", lambda e, cc=cc, pa=pa: e.activation(out=AS[:, cc, :], in_=pa[:, :], func=AF.Sigmoid, bias=a0_col[:, cc:cc + 1]), r=[bpa, Bv], w=[BAS])
                em.op("dve", lambda e: e.tensor_scalar(out=fl(LW), in0=fl(LW), scalar1=-DEC_C, scalar2=None, op0=ALU.mult), r=[BLW], w=[BLW])
                if l == 0:
                    for cc in range(4):
                        em.dma("sp", vfirst_d[cc, :, t0:t0 + 512], V_[:, cc, :], r=[BV_], w=[B_vf])
                else:
                    em.dma("sp", vl[:, :], rw_d[14, 0:32, t0:t0 + 512], r=[B_rw], w=[Blo])
                    for cc in range(4):
                        pv, bpv = nps()
                        em.op("pe", lambda e, cc=cc, pv=pv: e.matmul(pv[:, :], lhsT=v2_sb[0:32, cc * 128:(cc + 1) * 128], rhs=vl[0:32, :], start=True, stop=True), r=[Bv, Blo], w=[bpv])
                        em.op("act", lambda e, cc=cc, pv=pv: e.activation(out=T1[:, cc, :], in_=pv[:, :], func=AF.Sigmoid, bias=v0_col[:, cc:cc + 1]), r=[bpv, Bv], w=[BT1])
                        em.dma("sp", T2[:, cc, :], vfirst_d[cc, :, t0:t0 + 512], r=[B_vf], w=[BT2])
                    em.op("dve", lambda e: e.tensor_tensor(out=fl(T2), in0=fl(T2), in1=fl(V_), op=ALU.subtract), r=[BT2, BV_], w=[BT2])
                    em.op("dve", lambda e: e.tensor_tensor(out=fl(T2), in0=fl(T2), in1=fl(T1), op=ALU.mult), r=[BT2, BT1], w=[BT2])
                    em.op("dve", lambda e: e.tensor_tensor(out=fl(V_), in0=fl(V_), in1=fl(T2), op=ALU.add), r=[BT2, BV_], w=[BV_])
                for cc in range(4):
                    em.op("dve", lambda e, cc=cc: e.tensor_scalar(out=T1[:, cc, :], in0=K_[:, cc, :], scalar1=kk_col[:, cc:cc + 1], scalar2=None, op0=ALU.mult), r=[BK_, Bv], w=[BT1])
                em.op("dve", lambda e: e.tensor_tensor(out=fl(T2), in0=fl(T1), in1=fl(T1), op=ALU.mult), r=[BT1], w=[BT2])
                for cc in range(4):
                    pss, bpss = nps()
                    em.op("pe", lambda e, cc=cc, pss=pss: e.matmul(pss[:, :], lhsT=blk_f[:, :], rhs=T2[:, cc, :], start=True, stop=True), r=[Bc, BT2], w=[bpss])
                    em.op("act", lambda e, cc=cc, pss=pss: e.activation(out=T3[:, cc, :], in_=pss[:, :], func=AF.Sqrt), r=[bpss], w=[BT3])
                em.op("dve", lambda e: e.tensor_scalar(out=fl(T3), in0=fl(T3), scalar1=1e-12, scalar2=None, op0=ALU.max), r=[BT3], w=[BT3])
                em.op("dve", lambda e: e.reciprocal(out=fl(T3), in_=fl(T3)), r=[BT3], w=[BT3])
                em.op("dve", lambda e: e.tensor_tensor(out=fl(T1), in0=fl(T1), in1=fl(T3), op=ALU.mult), r=[BT1, BT3], w=[BT1])
                for cc in range(4):
                    em.op("dve", lambda e, cc=cc: e.tensor_scalar(out=T2[:, cc, :], in0=AS[:, cc, :], scalar1=-1.0, scalar2=ka_col[:, cc:cc + 1], op0=ALU.add, op1=ALU.mult), r=[BAS, Bv], w=[BT2])
                em.op("dve", lambda e: e.scalar_tensor_tensor(out=fl(K_), in0=fl(T2), scalar=1.0, in1=fl(K_), op0=ALU.add, op1=ALU.mult), r=[BT2, BK_], w=[BK_])
                for cc in range(4):
                    em.op("dve", lambda e, cc=cc: e.scalar_tensor_tensor(out=T3[:, cc, :], in0=R_[:, cc, :], scalar=rk_col[:, cc:cc + 1], in1=K_[:, cc, :], op0=ALU.mult, op1=ALU.mult), r=[BR, BK_, Bv], w=[BT3])
                pb, bpb = nps()
                for n in range(4):
                    for cc in range(4):
                        em.op("pe", lambda e, n=n, cc=cc: e.matmul(pb[:, n * 8 + cc * 2:n * 8 + cc * 2 + 2], lhsT=T3[:, cc, n * 128:(n + 1) * 128], rhs=hsel[:, :], start=True, stop=True), r=[BT3, Bc], w=[bpb])
                em.op("act", lambda e: e.activation(out=bonus[:, :], in_=pb[:, 0:32], func=AF.Copy), r=[bpb], w=[Bbon])
                em.op("dve", lambda e: e.tensor_tensor(out=fl(T2), in0=fl(T1), in1=fl(AS), op=ALU.mult), r=[BT1, BAS], w=[BT2])
                em.op("dve", lambda e: e.tensor_scalar(out=fl(T1), in0=fl(T1), scalar1=-1.0, scalar2=None, op0=ALU.mult), r=[BT1], w=[BT1])
                src, bsrc = LW, BLW
                for si, sft in enumerate((1, 2, 4, 8, 16, 32, 64)):
                    dst, bdst = (CA, BCA) if si % 2 == 0 else (CB, BCB)
                    em.op("act", lambda e, src=src, dst=dst, sft=sft: e.activation(out=v3(dst)[:, :, 0:sft], in_=v3(src)[:, :, 0:sft], func=AF.Copy), r=[bsrc], w=[bdst])
                    em.op("dve", lambda e, src=src, dst=dst, sft=sft: e.tensor_tensor(out=v3(dst)[:, :, sft:128], in0=v3(src)[:, :, sft:128], in1=v3(src)[:, :, 0:128 - sft], op=ALU.add), r=[bsrc], w=[bdst])
                    src, bsrc = dst, bdst
                assert src is CA
                em.op("act", lambda e: e.activation(out=fl(T3), in_=fl(CA), func=AF.Exp), r=[BCA], w=[BT3])
                em.op("dve", lambda e: e.tensor_tensor(out=fl(R_), in0=fl(R_), in1=fl(T3), op=ALU.mult), r=[BR, BT3], w=[BR])
                em.op("dve", lambda e: e.tensor_tensor(out=fl(CB), in0=fl(CA), in1=fl(LW), op=ALU.subtract), r=[BCA, BLW], w=[BCB])
                em.op("act", lambda e: e.activation(out=fl(T3), in_=fl(CB), func=AF.Exp), r=[BCB], w=[BT3])
                em.op("dve", lambda e: e.tensor_tensor(out=fl(T1), in0=fl(T1), in1=fl(T3), op=ALU.mult), r=[BT1, BT3], w=[BT1])
                em.op("act", lambda e: e.activation(out=fl(T3), in_=fl(CA), func=AF.Exp, scale=-1.0), r=[BCA], w=[BT3])
                em.op("dve", lambda e: e.tensor_tensor(out=fl(T4), in0=fl(T2), in1=fl(T3), op=ALU.mult), r=[BT2, BT3], w=[BT4])
                em.op("dve", lambda e: e.tensor_tensor(out=fl(T5), in0=fl(K_), in1=fl(T3), op=ALU.mult), r=[BK_, BT3], w=[BT5])
                em.op("dve", lambda e: e.tensor_tensor(out=v3(CB), in0=v3(CA)[:, :, 127:128].to_broadcast([128, 16, 128]), in1=v3(CA), op=ALU.subtract), r=[BCA], w=[BCB])
                em.op("act", lambda e: e.activation(out=fl(T3), in_=fl(CB), func=AF.Exp), r=[BCB], w=[BT3])
                em.op("dve", lambda e: e.tensor_tensor(out=fl(T2), in0=fl(T2), in1=fl(T3), op=ALU.mult), r=[BT2, BT3], w=[BT2])
                em.op("dve", lambda e: e.tensor_tensor(out=fl(K_), in0=fl(K_), in1=fl(T3), op=ALU.mult), r=[BK_, BT3], w=[BK_])
                em.op("act", lambda e: e.activation(out=pcs[:, :].unsqueeze(2), in_=v3(CA)[:, :, 127:128], func=AF.Exp), r=[BCA], w=[Bpc])
                em.op("dve", lambda e: e.tensor_scalar(out=fl(CB), in0=fl(T1), scalar1=hsel[:, 1:2], scalar2=None, op0=ALU.mult), r=[BT1, Bc, BCA], w=[BCB])
                em.op("dve", lambda e: e.tensor_scalar(out=fl(CA), in0=fl(T1), scalar1=hsel[:, 0:1], scalar2=None, op0=ALU.mult), r=[BT1, Bc, Bpc], w=[BCA])
                em.op("dve", lambda e: e.tensor_scalar(out=fl(LW), in0=fl(R_), scalar1=hsel[:, 0:1], scalar2=None, op0=ALU.mult), r=[BR, Bc], w=[BLW])
                em.op("dve", lambda e: e.tensor_scalar(out=fl(AS), in0=fl(R_), scalar1=hsel[:, 1:2], scalar2=None, op0=ALU.mult), r=[BR, Bc], w=[BAS])
                Am = ((CA, BCA), (CB, BCB)); Rm = ((LW, BLW), (AS, BAS))
                em.op("act", lambda e: e.activation(out=lorg[:, :], in_=lorg[:, :], func=AF.Sigmoid), r=[Blo], w=[Blo])

                if rstop <= 1:
                    continue
                for n in range(4):
                    if rstop < 99 and n > 0:
                        continue
                    ts_ = slice(n * 128, (n + 1) * 128)

                    def hs(t, h):
                        return t[(h % 2) * 64:(h % 2) * 64 + 64, h // 2, ts_]
                    import os as _os

                    def full(t, h):
                        return t[:, h // 2, ts_]
                    specs = (("N", "b", "A", mstrict), ("M", "A", "b", mstrictL), ("AkT", "k", "A", mstrict), ("GbT", "b", "R", mincl), ("GkT", "k", "R", mincl))

                    def opnd(code, h):
                        if code == "b":
                            return full(T4, h), BT4
                        if code == "k":
                            return full(T5, h), BT5
                        if code == "A":
                            return full(Am[h % 2][0], h), Am[h % 2][1]
                        return full(Rm[h % 2][0], h), Rm[h % 2][1]
                    for (nm, Lc, Rc, msk) in specs:
                        mt, bm = mats[nm]
                        for hb in range(2):
                            pm, bpm = nps()
                            for h4 in range(4):
                                h = hb * 4 + h4
                                La, Lb = opnd(Lc, h)
                                Ra, Rb = opnd(Rc, h)
                                em.op("pe", lambda e, h4=h4, pm=pm, La=La, Ra=Ra: e.matmul(pm[:, h4 * 128:(h4 + 1) * 128], lhsT=La, rhs=Ra, start=True, stop=True), r=[Lb, Rb], w=[bpm])
                            em.op("dve", lambda e, pm=pm, mt=mt, hb=hb, msk=msk: e.tensor_tensor(out=mt[:, hb * 4:(hb + 1) * 4, :], in0=pm[:, :].rearrange("p (a b) -> p a b", b=128), in1=msk[:, :].unsqueeze(1).to_broadcast([128, 4, 128]), op=ALU.mult), r=[bpm, Bc], w=[bm[hb]])
                    em.log = False
                    if rstop <= 2:
                        continue
                    Tt, bTt = mats["Tt"]
                    for hb in range(2):
                        em.op("dve", lambda e, hb=hb: e.tensor_tensor(out=Tt[:, hb * 4:(hb + 1) * 4, :], in0=mats["N"][0][:, hb * 4:(hb + 1) * 4, :], in1=ident_f[:, :].unsqueeze(1).to_broadcast([128, 4, 128]), op=ALU.add), r=[mats["N"][1][hb], Bc], w=[bTt[hb]])
                    Xc, XTc, Xn, XTn = "N", "M", "X2", "XT2"
                    for step in range(6):
                        lastst = (step == 5)
                        for hb in range(2):
                            if not lastst:
                                pX, bpX = nps()
                                for h4 in range(4):
                                    h = hb * 4 + h4
                                    em.op("pe", lambda e, h=h, h4=h4, pX=pX, Xc=Xc, XTc=XTc: e.matmul(pX[:, h4 * 128:(h4 + 1) * 128], lhsT=mats[XTc][0][:, h, :], rhs=mats[Xc][0][:, h, :], start=True, stop=True), r=[mats[XTc][1][hb], mats[Xc][1][hb]], w=[bpX])
                            pXT, bpXT = nps()
                            for h4 in range(4):
                                h = hb * 4 + h4
                                em.op("pe", lambda e, h=h, h4=h4, pXT=pXT, Xc=Xc, XTc=XTc: e.matmul(pXT[:, h4 * 128:(h4 + 1) * 128], lhsT=mats[Xc][0][:, h, :], rhs=mats[XTc][0][:, h, :], start=True, stop=True), r=[mats[XTc][1][hb], mats[Xc][1][hb]], w=[bpXT])
                            if not lastst:
                                em.op("act", lambda e, pX=pX, Xn=Xn, hb=hb: e.activation(out=mats[Xn][0][:, hb * 4:(hb + 1) * 4, :], in_=pX[:, :].rearrange("p (a b) -> p a b", b=128), func=AF.Copy), r=[bpX], w=[mats[Xn][1][hb]])
                            em.op("dve", lambda e, pXT=pXT, XTn=XTn, hb=hb: e.tensor_copy(mats[XTn][0][:, hb * 4:(hb + 1) * 4, :], pXT[:, :].rearrange("p (a b) -> p a b", b=128)), r=[bpXT], w=[mats[XTn][1][hb]])
                        for hb in range(2):
                            pT, bpT = nps()
                            for h4 in range(4):
                                h = hb * 4 + h4
                                em.op("pe", lambda e, h=h, h4=h4, pT=pT, XTn=XTn: e.matmul(pT[:, h4 * 128:(h4 + 1) * 128], lhsT=mats[XTn][0][:, h, :], rhs=Tt[:, h, :], start=True, stop=True), r=[mats[XTn][1][hb], bTt[hb]], w=[bpT])
                            em.op("dve", lambda e, pT=pT, hb=hb: e.tensor_tensor(out=Tt[:, hb * 4:(hb + 1) * 4, :], in0=pT[:, :].rearrange("p (a b) -> p a b", b=128), in1=Tt[:, hb * 4:(hb + 1) * 4, :], op=ALU.add), r=[bpT, bTt[hb]], w=[bTt[hb]])
                        Xc, XTc, Xn, XTn = Xn, XTn, Xc, XTc
                    if rstop <= 3:
                        continue
                    for (nm, src_t, bsrc_t) in (("Vtok", V_, BV_), ("Bhtok", T2, BT2), ("Khtok", K_, BK_)):
                        ptt, bptt = nps()
                        for cc in range(4):
                            em.op("pe", lambda e, cc=cc, ptt=ptt, src_t=src_t: e.transpose(ptt[:, cc * 128:(cc + 1) * 128], src_t[:, cc, ts_], ident_f[:, :]), r=[bsrc_t, Bc], w=[bptt])
                        em.op("act", lambda e, ptt=ptt, nm=nm: e.activation(out=tokt[nm][0][:, :], in_=ptt[:, :], func=AF.Copy), r=[bptt], w=[tokt[nm][1]])
                    Vtok, BVt = tokt["Vtok"]; Bhtok, BBh = tokt["Bhtok"]; Khtok, BKh = tokt["Khtok"]
                    Wsb, BW_ = tokt["Wsb"]; Usb, BU_ = tokt["Usb"]; Ysb, BY_ = tokt["Ysb"]
                    AkT, bAk = mats["AkT"]; GbT, bGb = mats["GbT"]; GkT, bGk = mats["GkT"]

                    def sst(h):
                        return Sst[:, h // 2, :]
                    if rstop <= 4:
                        continue
                    pW, bpW = nps()
                    for h in range(8):
                        em.op("pe", lambda e, h=h: e.matmul(pW[:, h * 64:(h + 1) * 64], lhsT=full(Am[h % 2][0], h), rhs=sst(h), start=True, stop=False), r=[Am[h % 2][1], BS], w=[bpW])
                        em.op("pe", lambda e, h=h: e.matmul(pW[:, h * 64:(h + 1) * 64], lhsT=AkT[:, h, :], rhs=Vtok[:, h * 64:(h + 1) * 64], start=False, stop=True), r=[bAk[h // 4], BVt], w=[bpW])
                    em.op("act", lambda e: e.activation(out=Wsb[:, :], in_=pW[:, :], func=AF.Copy), r=[bpW], w=[BW_])
                    pU, bpU = nps()
                    for h in range(8):
                        em.op("pe", lambda e, h=h: e.matmul(pU[:, h * 64:(h + 1) * 64], lhsT=Tt[:, h, :], rhs=Wsb[:, h * 64:(h + 1) * 64], start=True, stop=True), r=[bTt[h // 4], BW_], w=[bpU])
                    em.op("dve", lambda e: e.tensor_copy(Usb[:, :], pU[:, :]), r=[bpU], w=[BU_])
                    pY, bpY = nps()
                    for h in range(8):
                        em.op("pe", lambda e, h=h: e.matmul(pY[:, h * 64:(h + 1) * 64], lhsT=full(Rm[h % 2][0], h), rhs=sst(h), start=True, stop=False), r=[Rm[h % 2][1], BS], w=[bpY])
                        em.op("pe", lambda e, h=h: e.matmul(pY[:, h * 64:(h + 1) * 64], lhsT=GbT[:, h, :], rhs=Usb[:, h * 64:(h + 1) * 64], start=False, stop=False), r=[bGb[h // 4], BU_], w=[bpY])
                        em.op("pe", lambda e, h=h: e.matmul(pY[:, h * 64:(h + 1) * 64], lhsT=GkT[:, h, :], rhs=Vtok[:, h * 64:(h + 1) * 64], start=False, stop=True), r=[bGk[h // 4], BVt], w=[bpY])
                    em.op("act", lambda e: e.activation(out=Ysb[:, :], in_=pY[:, :], func=AF.Copy), r=[bpY], w=[BY_])
                    if rstop <= 5:
                        continue
                    pS, bpS = nps()
                    for cc in range(4):
                        o_ = pS[:, cc * 128:(cc + 1) * 128]
                        em.op("pe", lambda e, cc=cc, o_=o_: e.matmul(o_, lhsT=Bhtok[:, cc * 128:(cc + 1) * 128], rhs=Usb[:, cc * 128:(cc + 1) * 128], start=True, stop=False), r=[BBh, BU_], w=[bpS])
                        em.op("pe", lambda e, cc=cc, o_=o_: e.matmul(o_, lhsT=Khtok[:, cc * 128:(cc + 1) * 128], rhs=Vtok[:, cc * 128:(cc + 1) * 128], start=False, stop=True), r=[BKh, BVt], w=[bpS])
                    for cc in range(4):
                        for hp in range(2):
                            pr_ = slice(hp * 64, hp * 64 + 64)
                            em.op("dve", lambda e, cc=cc, hp=hp, pr_=pr_: e.scalar_tensor_tensor(out=Sst[pr_, cc, :], in0=Sst[pr_, cc, :], scalar=pcs[pr_, cc * 4 + n:cc * 4 + n + 1], in1=pS[pr_, cc * 128 + hp * 64:cc * 128 + hp * 64 + 64], op0=ALU.mult, op1=ALU.add), r=[BS, Bpc, bpS], w=[BS])
                    if rstop <= 6:
                        continue
                    cen, Bcen = tokt["cen"]; sq, Bsq_ = tokt["sq"]; gte, Bgte = tokt["gte"]
                    y3 = Ysb[:, :].rearrange("p (h i) -> p h i", i=64)
                    c3 = cen[:, :].rearrange("p (h i) -> p h i", i=64)
                    s3 = sq[:, :].rearrange("p (h i) -> p h i", i=64)
                    em.op("dve", lambda e: e.tensor_reduce(out=st8[0][:, :], in_=y3, axis=AX.X, op=ALU.add), r=[BY_], w=[Bst8])
                    em.op("dve", lambda e: e.tensor_scalar(out=st8[0][:, :], in0=st8[0][:, :], scalar1=1.0 / 64, scalar2=None, op0=ALU.mult), r=[Bst8], w=[Bst8])
                    em.op("dve", lambda e: e.tensor_tensor(out=c3, in0=y3, in1=st8[0][:, :].unsqueeze(2).to_broadcast([128, 8, 64]), op=ALU.subtract), r=[BY_, Bst8], w=[Bcen])
                    em.op("dve", lambda e: e.tensor_tensor(out=sq[:, :], in0=cen[:, :], in1=cen[:, :], op=ALU.mult), r=[Bcen], w=[Bsq_])
                    em.op("dve", lambda e: e.tensor_reduce(out=st8[1][:, :], in_=s3, axis=AX.X, op=ALU.add), r=[Bsq_], w=[Bst8])
                    em.op("act", lambda e: e.activation(out=st8[1][:, :], in_=st8[1][:, :], func=AF.Sqrt, bias=gneps[:, 0:1], scale=1.0 / 64), r=[Bst8, Bc], w=[Bst8])
                    em.op("dve", lambda e: e.reciprocal(out=st8[1][:, :], in_=st8[1][:, :]), r=[Bst8], w=[Bst8])
                    em.op("dve", lambda e: e.tensor_tensor(out=c3, in0=c3, in1=st8[1][:, :].unsqueeze(2).to_broadcast([128, 8, 64]), op=ALU.mult), r=[Bcen, Bst8], w=[Bcen])
                    em.op("dve", lambda e: e.tensor_tensor(out=cen[:, :], in0=cen[:, :], in1=lnw_bc[:, :], op=ALU.mult), r=[Bcen, Bv], w=[Bcen])
                    em.op("dve", lambda e: e.tensor_tensor(out=cen[:, :], in0=cen[:, :], in1=lnb_bc[:, :], op=ALU.add), r=[Bcen, Bv], w=[Bcen])
                    v3t = Vtok[:, :].rearrange("p (h i) -> p h i", i=64)
                    em.op("dve", lambda e: e.tensor_tensor(out=s3, in0=v3t, in1=bonus[:, n * 8:(n + 1) * 8].unsqueeze(2).to_broadcast([128, 8, 64]), op=ALU.mult), r=[BVt, Bbon], w=[Bsq_])
                    em.op("dve", lambda e: e.tensor_tensor(out=cen[:, :], in0=cen[:, :], in1=sq[:, :], op=ALU.add), r=[Bcen, Bsq_], w=[Bcen])
                    pG, bpG = nps()
                    em.op("pe", lambda e: e.matmul(pG[:, :], lhsT=lorg[:, ts_], rhs=lora_w[:, 1, :], start=True, stop=True), r=[Blo, Bv], w=[bpG])
                    em.op("dve", lambda e: e.tensor_tensor(out=outb[:, :], in0=cen[:, :], in1=pG[:, :], op=ALU.mult), r=[Bcen, bpG], w=[Boutb])
                    pO, bpO = nps()
                    pOb = pO[:, :].bitcast(BF16)
                    for cc in range(4):
                        em.op("pe", lambda e, cc=cc: e.transpose(pOb[:, cc * 128:(cc + 1) * 128], outb[:, cc * 128:(cc + 1) * 128], ident_b[:, :]), r=[Boutb, Bc], w=[bpO])
                    em.op("act", lambda e: e.activation(out=yTb[:, :, :], in_=pOb[:, 0:512].rearrange("p (c t) -> p c t", t=128), func=AF.Copy), r=[bpO], w=[ByTb])
                    tk0 = t0 + n * 128
                    em.dma("sp", ycat_d[4:8, :, tk0:tk0 + 128].rearrange("c p t -> p c t"), yTb[:, :, :], r=[ByTb], w=[B_ycat])
            em.barrier()
        if dbg and dbg[0] == "rwkv":
            break
        with ExitStack() as es:
            knT = sb(es, "knT", [128, 4, S], BF16); kpeT = sb(es, "kpeT", [65, S], BF16); Vsb = sb(es, "Vsb", [128, NT, 512], BF16)
            BK = Buf()
            for h in range(4):
                em.dma("sp", knT[:, h, :], kn_d[h], r=[B_k], w=[BK])
            em.dma("sp", kpeT[0:64, :], kpe_d, r=[B_k], w=[BK])
            em.op("dve", lambda e: e.memset(kpeT[64:65, :], -1.0), w=[BK])
            for j4 in range(4):
                em.dma("sp", Vsb[:, j4 * 8:(j4 + 1) * 8, :], v_d[j4 * 1024:(j4 + 1) * 1024, :].rearrange("(j p) n -> p j n", p=128), r=[B_k], w=[BK])
            gat_col = sb(es, "gat_col", [128, 4])
            em.dma("sp", gat_col[:, :], g_attn_out[l, :].rearrange("(k p) -> p k", p=128), w=[BK], allow_slow_non_contiguous=True)
            kmax2 = sb(es, "kmax2", [128, 4]); kmx = sb(es, "kmx", [128, 1]); Bkm = Buf()
            sq1 = sb(es, "sq1", [128, 512]); sq2 = sb(es, "sq2", [64, 512]); Bsq = Buf()
            em.op("dve", lambda e: e.memset(kmax2[:, :], 0.0), w=[Bkm])
            for g in range(NG):
                em.op("dve", lambda e, g=g: e.tensor_tensor(out=sq2[:, :], in0=kpeT[0:64, g * 512:(g + 1) * 512], in1=kpeT[0:64, g * 512:(g + 1) * 512], op=ALU.mult), r=[BK], w=[Bsq])
                for h in range(4):
                    em.op("dve", lambda e, g=g, h=h: e.tensor_tensor(out=sq1[:, :], in0=knT[:, h, g * 512:(g + 1) * 512], in1=knT[:, h, g * 512:(g + 1) * 512], op=ALU.mult), r=[BK], w=[Bsq])
                    pt, bp = ps[4 + (h % 4)], Bps[4 + (h % 4)]
                    em.op("pe", lambda e, pt=pt: e.matmul(pt[:, :], lhsT=ones_f[:, :], rhs=sq1[:, :], start=True, stop=False), r=[Bc, Bsq], w=[bp])
                    em.op("pe", lambda e, pt=pt: e.matmul(pt[:, :], lhsT=ones_f[0:64, :], rhs=sq2[:, :], start=False, stop=True), r=[Bc, Bsq], w=[bp])
                    em.op("dve", lambda e, pt=pt: e.tensor_reduce(out=kmx[:, :], in_=pt[:, :], axis=AX.X, op=ALU.max), r=[bp], w=[Bkm])
                    em.op("dve", lambda e, h=h: e.tensor_tensor(out=kmax2[:, h:h + 1], in0=kmax2[:, h:h + 1], in1=kmx[:, :], op=ALU.max), r=[Bkm], w=[Bkm])
            qn_s = [sb(es, "qn_s%d" % i, [128, 512], BF16) for i in range(2)]
            qpe_s = [sb(es, "qpe_s%d" % i, [65, 512], BF16) for i in range(2)]
            qsq_s = [sb(es, "qsq_s%d" % i, [65, 512]) for i in range(2)]
            Bqs_ = [Buf(), Buf()]
            PT = [sb(es, "PT%d" % i, [128, 512], BF16) for i in range(4)]; BPT = [Buf() for _ in range(4)]
            dacc = sb(es, "dacc", [128, 512]); Bdacc = Buf()
            oT = [sb(es, "oT%d" % i, [128, 512]) for i in range(4)]; BoT = [Buf() for _ in range(4)]
            rden = sb(es, "rden", [128, 512]); Brd = Buf()
            osq = sb(es, "osq", [128, 512]); Bosq = Buf()
            rs_a = sb(es, "rs_a", [128, 512]); Brs = Buf()
            yst = [sb(es, "yst%d" % i, [128, 512], BF16) for i in range(2)]; Byst = [Buf(), Buf()]
            cnt = 0
            for I in range(NG):
                q0 = I * 512
                for h in range(4):
                    qi = (I * 4 + h) % 2
                    em.dma("sp", qn_s[qi][:, :], qn_d[h, :, q0:q0 + 512], r=[B_q], w=[Bqs_[qi]])
                    em.dma("sp", qpe_s[qi][0:64, :], qpe_d[h, :, q0:q0 + 512], r=[B_q], w=[Bqs_[qi]])
                    em.dma("sp", qsq_s[qi][64:65, :], qsq_d[h:h + 1, q0:q0 + 512], r=[B_q], w=[Bqs_[qi]])
                    em.op("act", lambda e, qi=qi, h=h: e.activation(out=qpe_s[qi][64:65, :], in_=qsq_s[qi][64:65, :], func=AF.Sqrt, scale=kmax2[64:65, h:h + 1]), r=[Bqs_[qi], Bkm], w=[Bqs_[qi]])
                    po, bpo = ps[(h % 2) * 2], Bps[(h % 2) * 2]
                    pd, bpd = ps[(h % 2) * 2 + 1], Bps[(h % 2) * 2 + 1]
                    nj = 4 * I + 4

                    def emit_scores(j):
                        r_ = j - 4 * I
                        c0 = max(r_, 0) * 128
                        k_ = (cnt0 + j)
                        sc, bsc = ps[4 + k_ % 4], Bps[4 + k_ % 4]
                        em.op("pe", lambda e: e.matmul(sc[:, c0:512], lhsT=knT[:, h, j * 128:(j + 1) * 128], rhs=qn_s[qi][:, c0:512], start=True, stop=False), r=[BK, Bqs_[qi]], w=[bsc])
                        em.op("pe", lambda e: e.matmul(sc[:, c0:512], lhsT=kpeT[0:65, j * 128:(j + 1) * 128], rhs=qpe_s[qi][0:65, c0:512], start=False, stop=True), r=[BK, Bqs_[qi]], w=[bsc])
                    cnt0 = cnt
                    emit_scores(0)
                    for j in range(nj):
                        r_ = j - 4 * I
                        c0 = max(r_, 0) * 128
                        pi = cnt % 4
                        sc, bsc = ps[4 + cnt % 4], Bps[4 + cnt % 4]
                        cnt += 1
                        if j + 1 < nj:
                            emit_scores(j + 1)
                        em.op("act", lambda e, sc=sc, c0=c0, pi=pi: e.activation(out=PT[pi][:, c0:512], in_=sc[:, c0:512], func=AF.Exp, scale=SCALE), r=[bsc], w=[BPT[pi]])
                        if r_ >= 0:
                            em.op("dve", lambda e, c0=c0, pi=pi: e.tensor_tensor(out=PT[pi][:, c0:c0 + 128], in0=PT[pi][:, c0:c0 + 128], in1=mincl_b[:, :], op=ALU.mult), r=[BPT[pi], Bc], w=[BPT[pi]])
                        em.op("pe", lambda e, c0=c0, pi=pi, j=j: e.matmul(po[:, c0:512], lhsT=Vsb[:, j, h * 128:(h + 1) * 128], rhs=PT[pi][:, c0:512], start=(j == 0), stop=(j == nj - 1)), r=[BK, BPT[pi]], w=[bpo])
                        if j == 0:
                            em.op("dve", lambda e, pi=pi: e.tensor_copy(dacc[:, :], PT[pi][:, :]), r=[BPT[pi]], w=[Bdacc])
                        else:
                            em.op("dve", lambda e, c0=c0, pi=pi: e.tensor_tensor(out=dacc[:, c0:512], in0=dacc[:, c0:512], in1=PT[pi][:, c0:512], op=ALU.add), r=[BPT[pi], Bdacc], w=[Bdacc])
                    em.op("pe", lambda e: e.matmul(pd[:, :], lhsT=ones_f[:, :], rhs=dacc[:, :], start=True, stop=True), r=[Bc, Bdacc], w=[bpd])
                    em.op("dve", lambda e: e.reciprocal(out=rden[:, :], in_=pd[:, :]), r=[bpd], w=[Brd])
                    em.op("dve", lambda e, h=h: e.tensor_tensor(out=oT[h][:, :], in0=po[:, :], in1=rden[:, :], op=ALU.mult), r=[bpo, Brd], w=[BoT[h]])
                pn, bpn = ps[4], Bps[4]
                for h in range(4):
                    em.op("dve", lambda e, h=h: e.tensor_tensor(out=osq[:, :], in0=oT[h][:, :], in1=oT[h][:, :], op=ALU.mult), r=[BoT[h]], w=[Bosq])
                    em.op("pe", lambda e, h=h: e.matmul(pn[:, :], lhsT=ones_f[:, :], rhs=osq[:, :], start=(h == 0), stop=(h == 3)), r=[Bc, Bosq], w=[bpn])
                rstd_op(rs_a[:, :], pn[:, :], 1.0 / 512, [bpn], [Brs])
                for h in range(4):
                    yi = h % 2
                    em.op("dve", lambda e, h=h, yi=yi: e.scalar_tensor_tensor(out=yst[yi][:, :], in0=oT[h][:, :], scalar=gat_col[:, h:h + 1], in1=rs_a[:, :], op0=ALU.mult, op1=ALU.mult), r=[BoT[h], Brs, BK], w=[Byst[yi]])
                    em.dma("sp", ycat_d[h, :, q0:q0 + 512], yst[yi][:, :], r=[Byst[yi]], w=[B_ycat])
            em.barrier()
        if dbg and dbg[0] == "attn":
            break
        last = (l == depth - 1)
        with ExitStack() as es:
            wout_b = sb(es, "wout_b", [128, 8, D], BF16); Bwo = Buf()
            em.dma("pool", wout_b[:, :, :], w_out[l].rearrange("(k p) n -> p k n", p=128), w=[Bwo])
            wg_b = [sb(es, "wg_b%d" % i, [128, 8, 512], BF16) for i in range(2)]
            wu_b = [sb(es, "wu_b%d" % i, [128, 8, 512], BF16) for i in range(2)]
            wd_b = [sb(es, "wd_b%d" % i, [128, 4, D], BF16) for i in range(2)]
            Bwg = [Buf(), Buf()]; Bwu = [Buf(), Buf()]; Bwd = [Buf(), Buf()]
            TS = 1024
            NTT = TS // 128
            acc = [sb(es, "acc%d" % i, [128, D]) for i in range(NTT)]; Bacc = [Buf() for _ in range(NTT)]
            h2T = sb(es, "h2T", [128, 8, TS], BF16); Bh2 = Buf()
            ycs = [sb(es, "ycs%d" % i, [128, 8, 128], BF16) for i in range(2)]; Bycs = [Buf(), Buf()]
            junk = sb(es, "junk2", [128, D], BF16); ssum = sb(es, "ssum2", [128, 1]); rstd = sb(es, "rstd2", [128, 1])
            xn = sb(es, "xn2", [128, D], BF16); Bnt = Buf()
            actT = [sb(es, "actT%d" % i, [128, 4, TS], BF16) for i in range(2)]; Bact = [Buf(), Buf()]
            sil = [sb(es, "sil%d" % i, [128, 512]) for i in range(2)]; Bsil = [Buf(), Buf()]
            gfin_bc = sb(es, "gfin_bc", [128, D]); Bgf = Buf()
            em.dma("sp", gfin_bc[:, :], g_final[0:1, :].to_broadcast([128, D]), w=[Bgf])
            ost = [sb(es, "ost%d" % i, [128, D]) for i in range(2)]; Bost = [Buf(), Buf()]
            moe = (l % 2 == 1)
            if not moe:
                DFF = 2816
                experts = [(wbf["fg"], wbf["fu"], wbf["fd"], None)]
            else:
                DFF = 3584
                experts = [(wbf["eg"][e_], wbf["eu"][e_], wbf["ed"][e_], e_) for e_ in range(8)]
                h2f = sb(es, "h2f", [128, D]); h2fT = sb(es, "h2fT", [128, 8, 128]); Bh2f = Buf(); Bh2fT = Buf()
                wr_sb = sb(es, "wr_sb", [128, 8, 8]); br_sb = sb(es, "br_sb", [1, 8]); Bwr = Buf()
                em.dma("sp", wr_sb[:, :, :], w_router[0].rearrange("(k p) e -> p k e", p=128), w=[Bwr])
                em.dma("sp", br_sb[:, :], b_router[0:1, :], w=[Bwr])
                lg = [sb(es, "lg%d" % i, [128, 8]) for i in range(5)]; m12 = sb(es, "m12", [128, 4]); Blg = Buf()
                gates = sb(es, "gates", [128, 8 * NTT]); Bgates = Buf()
            groups = [(f0, min(512, DFF - f0)) for f0 in range(0, DFF, 512)]
            gcount = 0
            for g in range(S // TS):
                t0 = g * TS
                for tt in range(NTT):
                    yi = (g * NTT + tt) % 2
                    tk0 = t0 + tt * 128
                    em.dma("sp", ycs[yi][:, :, :], ycat_d[:, :, tk0:tk0 + 128].rearrange("c p t -> p c t"), r=[B_ycat], w=[Bycs[yi]])
                    em.dma("sp", acc[tt][:, :], x_src[tk0:tk0 + 128, :], r=[Bxsrc], w=[Bacc[tt]])
                    for half in range(2):
                        hs_ = slice(half * 512, (half + 1) * 512)
                        pt, bp = nps()
                        for k in range(8):
                            em.op("pe", lambda e, k=k, pt=pt, hs_=hs_: e.matmul(pt[:, :], lhsT=ycs[yi][:, k, :], rhs=wout_b[:, k, hs_], start=(k == 0), stop=(k == 7)), r=[Bycs[yi], Bwo], w=[bp])
                        em.op("dve", lambda e, pt=pt, hs_=hs_: e.tensor_tensor(out=ost[0][:, hs_], in0=pt[:, :], in1=gt1_bc[l][:, hs_], op=ALU.mult), r=[bp, Bmod], w=[Bost[0]])
                        em.op("dve", lambda e, hs_=hs_, tt=tt: e.tensor_tensor(out=acc[tt][:, hs_], in0=ost[0][:, hs_], in1=acc[tt][:, hs_], op=ALU.add), r=[Bost[0], Bacc[tt]], w=[Bacc[tt]])
                    rmsnorm_T(es, acc[tt], Bacc[tt], gs2[l], sh2_col, h2T, Bh2, tt * 128, (junk, ssum, rstd, xn, Bnt))
                    if moe:
                        em.op("dve", lambda e, tt=tt: e.scalar_tensor_tensor(out=h2f[:, :], in0=acc[tt][:, :], scalar=rstd[:, 0:1], in1=gs2_bc[:, :], op0=ALU.mult, op1=ALU.mult), r=[Bacc[tt], Bnt, Bmod], w=[Bh2f])
                        em.op("dve", lambda e: e.tensor_tensor(out=h2f[:, :], in0=h2f[:, :], in1=sh2_bc[:, :], op=ALU.add), r=[Bh2f, Bmod], w=[Bh2f])
                        for hf in range(2):
                            pT_, bpT_ = nps()
                            for k4 in range(4):
                                k = hf * 4 + k4
                                em.op("pe", lambda e, k=k, k4=k4, pT_=pT_: e.transpose(pT_[:, k4 * 128:(k4 + 1) * 128], h2f[:, k * 128:(k + 1) * 128], ident_f[:, :]), r=[Bh2f, Bc], w=[bpT_])
                            em.op("act", lambda e, hf=hf, pT_=pT_: e.activation(out=h2fT[:, hf * 4:(hf + 1) * 4, :], in_=pT_[:, :].rearrange("p (a b) -> p a b", b=128), func=AF.Copy), r=[bpT_], w=[Bh2fT])
                        pl, bpl = nps()
                        for k in range(8):
                            em.op("pe", lambda e, k=k: e.matmul(pl[:, 0:8], lhsT=h2fT[:, k, :], rhs=wr_sb[:, k, :], start=(k == 0), stop=False), r=[Bh2fT, Bwr], w=[bpl])
                        em.op("pe", lambda e: e.matmul(pl[:, 0:8], lhsT=ones_f[0:1, 0:128], rhs=br_sb[0:1, 0:8], start=False, stop=True), r=[Bc, Bwr], w=[bpl])
                        L0, L2, MK1, MK2, TMP = lg
                        em.op("act", lambda e: e.activation(out=L0[:, :], in_=pl[:, 0:8], func=AF.Copy), r=[bpl], w=[Blg])
                        em.op("dve", lambda e: e.tensor_reduce(out=m12[:, 0:1], in_=L0[:, :], axis=AX.X, op=ALU.max), r=[Blg], w=[Blg])
                        em.op("dve", lambda e: e.tensor_scalar(out=MK1[:, :], in0=L0[:, :], scalar1=m12[:, 0:1], scalar2=None, op0=ALU.is_equal), r=[Blg], w=[Blg])
                        em.op("dve", lambda e: e.scalar_tensor_tensor(out=L2[:, :], in0=MK1[:, :], scalar=-1e30, in1=L0[:, :], op0=ALU.mult, op1=ALU.add), r=[Blg], w=[Blg])
                        em.op("dve", lambda e: e.tensor_reduce(out=m12[:, 1:2], in_=L2[:, :], axis=AX.X, op=ALU.max), r=[Blg], w=[Blg])
                        em.op("dve", lambda e: e.tensor_scalar(out=MK2[:, :], in0=L2[:, :], scalar1=m12[:, 1:2], scalar2=None, op0=ALU.is_equal), r=[Blg], w=[Blg])
                        em.op("dve", lambda e: e.tensor_tensor(out=m12[:, 2:3], in0=m12[:, 1:2], in1=m12[:, 0:1], op=ALU.subtract), r=[Blg], w=[Blg])
                        em.op("act", lambda e: e.activation(out=m12[:, 3:4], in_=m12[:, 2:3], func=AF.Sigmoid), r=[Blg], w=[Blg])
                        em.op("act", lambda e: e.activation(out=m12[:, 2:3], in_=m12[:, 2:3], func=AF.Sigmoid, scale=-1.0), r=[Blg], w=[Blg])
                        em.op("dve", lambda e: e.tensor_scalar(out=TMP[:, :], in0=MK1[:, :], scalar1=m12[:, 2:3], scalar2=None, op0=ALU.mult), r=[Blg], w=[Blg])
                        em.op("dve", lambda e, tt=tt: e.scalar_tensor_tensor(out=gates[:, tt * 8:(tt + 1) * 8], in0=MK2[:, :], scalar=m12[:, 3:4], in1=TMP[:, :], op0=ALU.mult, op1=ALU.add), r=[Blg], w=[Bgates])
                for (wG, wU, wD, eidx) in experts:
                    for (f0, fw) in groups:
                        wi = gcount % 2
                        gcount += 1
                        nfc = fw // 128
                        em.dma("sp", wg_b[wi][:, :, 0:fw], wG[:, f0:f0 + fw].rearrange("(k p) n -> p k n", p=128), r=[Bwbf], w=[Bwg[wi]])
                        em.dma("pool", wu_b[wi][:, :, 0:fw], wU[:, f0:f0 + fw].rearrange("(k p) n -> p k n", p=128), r=[Bwbf], w=[Bwu[wi]])
                        em.dma("sp", wd_b[wi][:, 0:nfc, :], wD[f0:f0 + fw, :].rearrange("(c p) n -> p c n", p=128), r=[Bwbf], w=[Bwd[wi]])
                        ai = wi
                        for c in range(nfc):
                            for nh in range(TS // 512):
                                ns_ = slice(nh * 512, (nh + 1) * 512)
                                pg, bpg = nps()
                                pu, bpu = nps()
                                for k in range(8):
                                    em.op("pe", lambda e, k=k, c=c, pg=pg, ns_=ns_: e.matmul(pg[:, :], lhsT=wg_b[wi][:, k, c * 128:(c + 1) * 128], rhs=h2T[:, k, ns_], start=(k == 0), stop=(k == 7)), r=[Bwg[wi], Bh2], w=[bpg])
                                for k in range(8):
                                    em.op("pe", lambda e, k=k, c=c, pu=pu, ns_=ns_: e.matmul(pu[:, :], lhsT=wu_b[wi][:, k, c * 128:(c + 1) * 128], rhs=h2T[:, k, ns_], start=(k == 0), stop=(k == 7)), r=[Bwu[wi], Bh2], w=[bpu])
                                si = (c * 2 + nh) % 2
                                em.op("act", lambda e, pg=pg, si=si: e.activation(out=sil[si][:, :], in_=pg[:, :], func=AF.Silu), r=[bpg], w=[Bsil[si]])
                                em.op("dve", lambda e, pu=pu, si=si, c=c, ns_=ns_: e.tensor_tensor(out=actT[ai][:, c, ns_], in0=sil[si][:, :], in1=pu[:, :], op=ALU.mult), r=[Bsil[si], bpu], w=[Bact[ai]])
                        for tt in range(NTT):
                            for half in range(2):
                                hs_ = slice(half * 512, (half + 1) * 512)
                                pt, bp = nps()
                                for c in range(nfc):
                                    em.op("pe", lambda e, c=c, pt=pt, tt=tt, hs_=hs_: e.matmul(pt[:, :], lhsT=actT[ai][:, c, tt * 128:(tt + 1) * 128], rhs=wd_b[wi][:, c, hs_], start=(c == 0), stop=(c == nfc - 1)), r=[Bact[ai], Bwd[wi]], w=[bp])
                                ti = (tt * 2 + half) % 2
                                if eidx is None:
                                    em.op("dve", lambda e, pt=pt, hs_=hs_, ti=ti: e.tensor_tensor(out=ost[ti][:, 0:512], in0=pt[:, :], in1=gt2_bc[l][:, hs_], op=ALU.mult), r=[bp, Bmod], w=[Bost[ti]])
                                else:
                                    em.op("dve", lambda e, pt=pt, hs_=hs_, ti=ti, tt=tt: e.scalar_tensor_tensor(out=ost[ti][:, 0:512], in0=pt[:, :], scalar=gates[:, tt * 8 + eidx:tt * 8 + eidx + 1], in1=gt2_bc[l][:, hs_], op0=ALU.mult, op1=ALU.mult), r=[bp, Bmod, Bgates], w=[Bost[ti]])
                                em.op("dve", lambda e, hs_=hs_, ti=ti, tt=tt: e.tensor_tensor(out=acc[tt][:, hs_], in0=ost[ti][:, 0:512], in1=acc[tt][:, hs_], op=ALU.add), r=[Bost[ti], Bacc[tt]], w=[Bacc[tt]])
                for tt in range(NTT):
                    tk0 = t0 + tt * 128
                    if not last:
                        em.dma("sp", xres[tk0:tk0 + 128, :], acc[tt][:, :], r=[Bacc[tt]], w=[B_xres])
                    else:
                        oi = tt % 2
                        em.op("act", lambda e, tt=tt: e.activation(out=junk[:, :], in_=acc[tt][:, :], func=AF.Square, accum_out=ssum[:, :]), r=[Bacc[tt]], w=[Bnt])
                        rstd_op(rstd[:, :], ssum[:, :], 1.0 / D, [Bnt], [Bnt])
                        em.op("dve", lambda e, tt=tt, oi=oi: e.scalar_tensor_tensor(out=ost[oi][:, :], in0=acc[tt][:, :], scalar=rstd[:, 0:1], in1=gfin_bc[:, :], op0=ALU.mult, op1=ALU.mult), r=[Bacc[tt], Bnt, Bgf], w=[Bost[oi]])
                        em.dma("sp", out_d[tk0:tk0 + 128, :], ost[oi][:, :], r=[Bost[oi]], w=[B_out])
            em.barrier()

    if dbg:
        em.barrier()
        srcs = {"yrw0": ycat_d[4], "yrw3": ycat_d[7], "ymla": ycat_d[0], "ymla3": ycat_d[3], "qn": qn_d[0], "qpe": qpe_d[0], "kn": kn_d[0], "kpe": kpe_d, "v": v_d, "qsq": qsq_d}
        for nm in dbg[2]:
            src = srcs[nm]
            dd = nc.dram_tensor("dbg_" + nm, list(src.shape), src.dtype, kind="ExternalOutput").ap()
            em.dma("sp", dd, src, w=[B_dbg])
    em.barrier()
    return nc


def _consts():
    i = np.arange(128)
    half = 32
    inv_freq = (10000.0 ** (-(np.arange(half, dtype=np.float32) / np.float32(half)))).astype(np.float32)
    return {
        "c_ident": np.eye(128, dtype=np.float32),
        "c_mincl": (i[:, None] <= i[None, :]).astype(np.float32),
        "c_mstrict": (i[:, None] < i[None, :]).astype(np.float32),
        "c_blk": ((i[:, None] // 64) == (i[None, :] // 64)).astype(np.float32),
        "c_mstrictL": (i[:, None] > i[None, :]).astype(np.float32),
        "c_hsel": ((i[:, None] // 64) == np.arange(2)[None, :]).astype(np.float32),
        "c_invf": np.concatenate([inv_freq, inv_freq]).reshape(64, 1).astype(np.float32),
        "c_sgn": np.concatenate([-np.ones(32), np.ones(32)]).reshape(64, 1).astype(np.float32),
    }


def _in_maps(inputs, depth=DEPTH):
    f = lambda a: np.ascontiguousarray(a)
    shared = {}
    for k in ("w_ada", "b_ada", "g_norm_mix", "g_norm_ffn", "w_in", "w_in_vres", "g_q_norm", "w_uq", "g_kv_norm",
              "w_ukv", "g_attn_out", "mu_shift", "mu_shift_vres", "w0", "w2", "a0", "a2", "g2", "v0", "v2", "k_k",
              "k_a", "ln_x_w", "ln_x_b", "w_out", "w_ffn_gate", "w_ffn_up", "w_ffn_down"):
        shared[k] = f(inputs[k])
    shared["r_k"] = f(np.asarray(inputs["r_k"]).reshape(2, 512))
    shared["g_final"] = f(np.asarray(inputs["g_final"]).reshape(1, D))
    if depth > 1:
        shared["w_router"] = f(inputs["w_router"])
        shared["b_router"] = f(inputs["b_router"])
        shared["w_exp_gate"] = f(np.asarray(inputs["w_exp_gate"])[0])
        shared["w_exp_up"] = f(np.asarray(inputs["w_exp_up"])[0])
        shared["w_exp_down"] = f(np.asarray(inputs["w_exp_down"])[0])
    shared.update(_consts())
    maps = []
    for b in range(8):
        m = dict(shared)
        m["x"] = f(np.asarray(inputs["x"])[b])
        m["c"] = f(np.asarray(inputs["c"])[b:b + 1])
        m["positions"] = f(np.asarray(inputs["positions"])[b:b + 1].astype(np.int32))
        maps.append(m)
    return maps


def kernel(**inputs):
    nc = build()
    maps = _in_maps(inputs)
    res = run_bass_kernel_spmd(nc, maps, core_ids=list(range(8)))
    return np.stack([np.asarray(r["out"]) for r in res.results], axis=0).astype(np.float32)
```
